# Optimizing a Trainium2 kernel written in Bass

```python
import jax, jax.numpy as jnp
from jax import lax
import numpy as np

D_MODEL = 1024
BATCH = 8
SEQ = 2048
DEPTH = 1

GRID_W = 64
NA_HEADS = 8
NA_HEAD_DIM = 64
NA_WIN_ROWS_MAX = 8
NA_WIN_COLS = 16
NA_WIDTH = NA_HEADS * NA_HEAD_DIM
MLA_HEADS = 8
MLA_Q_RANK = 256
MLA_KV_RANK = 128
MLA_NOPE_DIM = 64
MLA_ROPE_DIM = 32
MLA_V_DIM = 64
MLA_QK_DIM = MLA_NOPE_DIM + MLA_ROPE_DIM
MLA_WIDTH = MLA_HEADS * MLA_V_DIM
ROPE_THETA = 10000.0
ATTN_BLOCK = 128
IN_SPLITS = (NA_WIDTH, NA_WIDTH, NA_WIDTH, MLA_Q_RANK, MLA_KV_RANK, MLA_ROPE_DIM, D_MODEL, D_MODEL)
IN_COLS = 3 * NA_WIDTH + MLA_Q_RANK + MLA_KV_RANK + MLA_ROPE_DIM + 2 * D_MODEL
N_EXPERTS = 64
TOP_K = 8
N_GROUPS = 8
TOPK_GROUPS = 4
EXPERT_FF = 256
SHARED_FF = 256
ROUTED_SCALE = 2.5
MOE_CHUNK = 128
EPS = 1e-6
NEG_BIG = -1e30

kernel_name = "hybrid_na_mla_moe_encoder_block"


def rms_norm(x, g):
    xf = x.astype(jnp.float32)
    y = xf * lax.rsqrt(jnp.mean(xf * xf, axis=-1, keepdims=True) + EPS)
    return (y * g.astype(jnp.float32)).astype(x.dtype)


def apply_rope(x, positions):
    half = x.shape[-1] // 2
    inv_freq = ROPE_THETA ** (-jnp.arange(half, dtype=jnp.float32) / half)
    ang = positions.astype(jnp.float32)[:, :, None, None] * inv_freq
    cos, sin = jnp.cos(ang), jnp.sin(ang)
    xf = x.astype(jnp.float32)
    x1, x2 = xf[..., :half], xf[..., half:]
    return jnp.concatenate([x1 * cos - x2 * sin, x2 * cos + x1 * sin], axis=-1).astype(x.dtype)


def neighbourhood_attention(q, k, v, rpb):
    B, S, H, Dh = q.shape
    R = S // GRID_W
    kh = min(NA_WIN_ROWS_MAX, R)
    to_grid = lambda t: t.reshape(B, R, GRID_W, H, Dh).transpose(0, 3, 1, 2, 4)
    qg, kg, vg = to_grid(q), to_grid(k), to_grid(v)
    rows = jnp.arange(R)
    row_start = jnp.clip(rows - kh // 2, 0, R - kh)
    key_rows = row_start[:, None] + jnp.arange(kh)[None, :]
    kb = kg[:, :, key_rows]
    vb = vg[:, :, key_rows]
    s = jnp.einsum('bhrqd,bhrjkd->bhrqjk', qg, kb).astype(jnp.float32) * (Dh ** -0.5)
    cols = jnp.arange(GRID_W)
    col_start = jnp.clip(cols - NA_WIN_COLS // 2, 0, GRID_W - NA_WIN_COLS)
    in_win = (cols[None, :] >= col_start[:, None]) & (cols[None, :] < col_start[:, None] + NA_WIN_COLS)
    dr = key_rows - rows[:, None]
    dc = jnp.clip(cols[None, :] - cols[:, None], -(NA_WIN_COLS - 1), NA_WIN_COLS - 1)
    bias = rpb[:, dr[:, None, :, None] + (NA_WIN_ROWS_MAX - 1),
               dc[None, :, None, :] + (NA_WIN_COLS - 1)]
    s = s + bias.astype(jnp.float32)[None]
    s = jnp.where(in_win[:, None, :], s, NEG_BIG)
    p = jax.nn.softmax(s.reshape(B, H, R, GRID_W, kh * GRID_W), axis=-1)
    p = p.reshape(B, H, R, GRID_W, kh, GRID_W).astype(v.dtype)
    o = jnp.einsum('bhrqjk,bhrjkd->bhrqd', p, vb)
    return o.transpose(0, 2, 3, 1, 4).reshape(B, S, H * Dh)


def blocked_full_attention(q, k, v):
    B, S, H, Dqk = q.shape
    Dv = v.shape[-1]
    nb = S // ATTN_BLOCK
    qb = q.reshape(B, nb, ATTN_BLOCK, H, Dqk).transpose(1, 0, 2, 3, 4)
    scale = Dqk ** -0.5

    def one_block(q_blk):
        s = jnp.einsum('bqhd,bkhd->bhqk', q_blk, k).astype(jnp.float32) * scale
        p = jax.nn.softmax(s, axis=-1).astype(v.dtype)
        return jnp.einsum('bhqk,bkhd->bqhd', p, v)

    o = lax.map(one_block, qb)
    return o.transpose(1, 0, 2, 3, 4).reshape(B, S, H * Dv)


def moe_ffn(h, w_router, e_bias, w_eg, w_eu, w_ed, w_sg, w_su, w_sd):
    B, S, D = h.shape
    t = h.reshape(-1, D)
    T = t.shape[0]
    scores = jax.nn.sigmoid((t @ w_router).astype(jnp.float32))
    sel = scores + e_bias.astype(jnp.float32)
    grp = sel.reshape(T, N_GROUPS, N_EXPERTS // N_GROUPS)
    grp_score = jnp.sum(lax.top_k(grp, 2)[0], axis=-1)
    _, gidx = lax.top_k(grp_score, TOPK_GROUPS)
    gmask = jnp.sum(jax.nn.one_hot(gidx, N_GROUPS, dtype=jnp.float32), axis=-2)
    emask = jnp.repeat(gmask, N_EXPERTS // N_GROUPS, axis=-1)
    sel = jnp.where(emask > 0, sel, -jnp.inf)
    _, eidx = lax.top_k(sel, TOP_K)
    w = jnp.take_along_axis(scores, eidx, axis=-1)
    w = w / jnp.sum(w, axis=-1, keepdims=True) * ROUTED_SCALE
    gates = jnp.sum(jax.nn.one_hot(eidx, N_EXPERTS, dtype=jnp.float32) * w[..., None], axis=-2)
    tc = t.reshape(-1, MOE_CHUNK, D)
    gc = gates.reshape(-1, MOE_CHUNK, N_EXPERTS).astype(t.dtype)

    def expert_chunk(args):
        xc, gch = args
        hg = jnp.einsum('cd,edf->cef', xc, w_eg)
        hu = jnp.einsum('cd,edf->cef', xc, w_eu)
        a = jax.nn.silu(hg) * hu * gch[..., None]
        return jnp.einsum('cef,efd->cd', a, w_ed)

    routed = lax.map(expert_chunk, (tc, gc)).reshape(T, D)
    shared = (jax.nn.silu(t @ w_sg) * (t @ w_su)) @ w_sd
    return (routed + shared).reshape(B, S, D)


def setup_inputs(seed: int = 0) -> dict:
    key = jax.random.key(seed)
    ks = iter(jax.random.split(key, 32))
    f32 = jnp.float32
    nrm = lambda shape, s: jax.random.normal(next(ks), shape, f32) * s
    gain = lambda shape: 1.0 + 0.05 * jax.random.normal(next(ks), shape, f32)
    L, D, E, F = DEPTH, D_MODEL, N_EXPERTS, EXPERT_FF
    x = jax.random.normal(next(ks), (BATCH, SEQ, D), f32)
    c = jax.random.normal(next(ks), (BATCH, D), f32)
    positions = jnp.broadcast_to(jnp.arange(SEQ, dtype=jnp.int32)[None, :], (BATCH, SEQ))
    return {
        "x": x,
        "c": c,
        "positions": positions,
        "w_ada": nrm((L, D, 6 * D), 0.3 * D ** -0.5),
        "b_ada": nrm((L, 6 * D), 0.02),
        "g_norm1": gain((L, D)),
        "w_in": nrm((L, D, IN_COLS), D ** -0.5),
        "g_na_q": gain((L, NA_HEAD_DIM)),
        "g_na_k": gain((L, NA_HEAD_DIM)),
        "na_rpb": nrm((L, NA_HEADS, 2 * NA_WIN_ROWS_MAX - 1, 2 * NA_WIN_COLS - 1), 0.1),
        "g_q_lat": gain((L, MLA_Q_RANK)),
        "w_uq": nrm((L, MLA_Q_RANK, MLA_HEADS * MLA_QK_DIM), MLA_Q_RANK ** -0.5),
        "g_kv_lat": gain((L, MLA_KV_RANK)),
        "w_ukv": nrm((L, MLA_KV_RANK, MLA_HEADS * (MLA_NOPE_DIM + MLA_V_DIM)), MLA_KV_RANK ** -0.5),
        "g_mla_q": gain((L, MLA_QK_DIM)),
        "g_mla_k": gain((L, MLA_QK_DIM)),
        "w_proj_na": nrm((L, NA_WIDTH, D), NA_WIDTH ** -0.5),
        "w_proj_mla": nrm((L, MLA_WIDTH, D), MLA_WIDTH ** -0.5),
        "w_out": nrm((L, D, D), D ** -0.5),
        "g_norm2": gain((L, D)),
        "w_router": nrm((L, D, E), D ** -0.5),
        "e_bias": nrm((L, E), 0.01),
        "w_exp_gate": nrm((L, E, D, F), D ** -0.5),
        "w_exp_up": nrm((L, E, D, F), D ** -0.5),
        "w_exp_down": nrm((L, E, F, D), F ** -0.5),
        "w_sh_gate": nrm((L, D, SHARED_FF), D ** -0.5),
        "w_sh_up": nrm((L, D, SHARED_FF), D ** -0.5),
        "w_sh_down": nrm((L, SHARED_FF, D), SHARED_FF ** -0.5),
    }


def reference(x, c, positions, w_ada, b_ada, g_norm1, w_in, g_na_q, g_na_k, na_rpb,
              g_q_lat, w_uq, g_kv_lat, w_ukv, g_mla_q, g_mla_k, w_proj_na, w_proj_mla,
              w_out, g_norm2, w_router, e_bias, w_exp_gate, w_exp_up, w_exp_down,
              w_sh_gate, w_sh_up, w_sh_down):
    B, S, D = x.shape
    split_at = list(np.cumsum(IN_SPLITS)[:-1])
    for l in range(DEPTH):
        mod = jax.nn.silu(c) @ w_ada[l] + b_ada[l]
        shift1, scale1, gate1, shift2, scale2, gate2 = jnp.split(mod[:, None, :], 6, axis=-1)

        h = rms_norm(x, g_norm1[l]) * (1.0 + scale1) + shift1
        proj = h @ w_in[l]
        na_q, na_k, na_v, q_lat, kv_lat, k_rot, gate_na, gate_mla = jnp.split(proj, split_at, axis=-1)

        qa = rms_norm(na_q.reshape(B, S, NA_HEADS, NA_HEAD_DIM), g_na_q[l])
        ka = rms_norm(na_k.reshape(B, S, NA_HEADS, NA_HEAD_DIM), g_na_k[l])
        va = na_v.reshape(B, S, NA_HEADS, NA_HEAD_DIM)
        y_na = neighbourhood_attention(qa, ka, va, na_rpb[l])

        qm = (rms_norm(q_lat, g_q_lat[l]) @ w_uq[l]).reshape(B, S, MLA_HEADS, MLA_QK_DIM)
        q_nope, q_rope = qm[..., :MLA_NOPE_DIM], apply_rope(qm[..., MLA_NOPE_DIM:], positions)
        kvm = (rms_norm(kv_lat, g_kv_lat[l]) @ w_ukv[l]).reshape(B, S, MLA_HEADS, MLA_NOPE_DIM + MLA_V_DIM)
        k_nope, vm = kvm[..., :MLA_NOPE_DIM], kvm[..., MLA_NOPE_DIM:]
        k_rope = jnp.broadcast_to(apply_rope(k_rot[:, :, None, :], positions), (B, S, MLA_HEADS, MLA_ROPE_DIM))
        qm = rms_norm(jnp.concatenate([q_nope, q_rope], axis=-1), g_mla_q[l])
        km = rms_norm(jnp.concatenate([k_nope, k_rope], axis=-1), g_mla_k[l])
        y_mla = blocked_full_attention(qm, km, vm)

        merged = (jax.nn.sigmoid(gate_na) * (y_na @ w_proj_na[l])
                  + jax.nn.sigmoid(gate_mla) * (y_mla @ w_proj_mla[l]))
        x = x + gate1 * (merged @ w_out[l])

        h2 = rms_norm(x, g_norm2[l]) * (1.0 + scale2) + shift2
        y_ffn = moe_ffn(h2, w_router[l], e_bias[l], w_exp_gate[l], w_exp_up[l], w_exp_down[l],
                        w_sh_gate[l], w_sh_up[l], w_sh_down[l])
        x = x + gate2 * y_ffn
    return x
```

```python
import numpy as np
from contextlib import ExitStack
import concourse.bass as bass
import concourse.mybir as mybir
from concourse.bass_utils import run_bass_kernel_spmd

F32 = mybir.dt.float32
BF16 = mybir.dt.bfloat16
I32 = mybir.dt.int32
ALU = mybir.AluOpType
AF = mybir.ActivationFunctionType
AX = mybir.AxisListType

D = 1024
S_ = 2048
NT = 16
NC4 = 4
E = 64
EPS = 1e-6
NEG = -30000.0
IN_COLS = 4000
C_Q, C_K, C_V, C_QL, C_KVL, C_KR, C_GN, C_GM = 0, 512, 1024, 1536, 1792, 1920, 1952, 2976

ENGS = ("pe", "act", "dve", "pool", "sp")


class _Op:
    __slots__ = ("eng", "fn", "reads", "writes", "dma_key", "waits", "inc", "tok", "idx", "bar")


class Sched:
    def __init__(self, nc):
        self.nc = nc
        self.ops = []
        self._cap = None

    def op(self, eng, fn, reads=(), writes=(), dma_key=None):
        o = _Op()
        o.eng, o.fn, o.reads, o.writes, o.dma_key = eng, fn, tuple(reads), tuple(writes), dma_key
        o.bar = False
        if self._cap is not None:
            self._cap.append(o)
            return o
        o.idx = len(self.ops)
        self.ops.append(o)
        return o

    def begin_unit(self):
        self._cap = []

    def end_unit(self):
        u, self._cap = self._cap, None
        return u

    def emit_units(self, units, shift=None, extra=()):
        n = max(len(u) for u in units)
        shift = shift or (n + 1) // 2
        T = (len(units) - 1) * shift + n
        extra = list(extra)
        every = max(1, T // (len(extra) + 1)) if extra else 0
        for t in range(T):
            for i, u in enumerate(units):
                p = t - i * shift
                if 0 <= p < len(u):
                    o = u[p]
                    o.idx = len(self.ops)
                    self.ops.append(o)
            if extra and t % every == every - 1:
                for o in extra.pop(0):
                    o.idx = len(self.ops)
                    self.ops.append(o)
        for u in extra:
            for o in u:
                o.idx = len(self.ops)
                self.ops.append(o)

    def barrier(self, skip=()):
        o = _Op()
        o.eng, o.fn, o.reads, o.writes, o.dma_key = None, None, (), tuple(skip), None
        o.bar = True
        o.idx = len(self.ops)
        self.ops.append(o)

    def finalize(self, es):
        nc = self.nc
        ops = self.ops
        last_w, readers = {}, {}
        last_comp, last_dma = {}, {}
        pending = {e: set() for e in ENGS}
        deps = [None] * len(ops)
        for o in ops:
            if o.bar:
                allp = set(last_comp.values()) | set(v for k, v in last_dma.items() if k not in o.writes)
                for e in ENGS:
                    pending[e] |= allp
                continue
            d = set()
            for r in o.reads:
                if r in last_w:
                    d.add(last_w[r])
            for w in o.writes:
                if w in last_w:
                    d.add(last_w[w])
                for rr in readers.get(w, ()):
                    d.add(rr)
            d |= pending[o.eng]
            pending[o.eng] = set()
            d.discard(o.idx)
            deps[o.idx] = d
            for r in o.reads:
                readers.setdefault(r, []).append(o.idx)
            for w in o.writes:
                last_w[w] = o.idx
                readers[w] = []
            if o.dma_key is not None:
                last_dma[o.dma_key] = o.idx
            else:
                last_comp[o.eng] = o.idx
        need_inc = [False] * len(ops)
        fdeps = [None] * len(ops)
        for o in ops:
            if o.bar:
                continue
            fd = []
            for pi in deps[o.idx]:
                p = ops[pi]
                if p.dma_key is None and o.dma_key is None and p.eng == o.eng and p.eng == "pe":
                    continue
                fd.append(pi)
                need_inc[pi] = True
            fdeps[o.idx] = fd
        sems, counts = {}, {}

        def get_sem(name):
            if name not in sems:
                sems[name] = es.enter_context(nc.semaphore(name))
                counts[name] = 0

        for o in ops:
            if o.bar:
                continue
            if o.dma_key is not None:
                name = "d_" + o.dma_key
                get_sem(name)
                counts[name] += 16
                o.tok, o.inc = (name, counts[name]), True
            elif need_inc[o.idx]:
                name = "e_" + o.eng
                get_sem(name)
                counts[name] += 1
                o.tok, o.inc = (name, counts[name]), True
            else:
                o.tok, o.inc = None, False
        waited = {e: {} for e in ENGS}
        for o in ops:
            if o.bar:
                continue
            need = {}
            for pi in fdeps[o.idx]:
                name, val = ops[pi].tok
                if need.get(name, 0) < val:
                    need[name] = val
            w = []
            for name, val in need.items():
                if waited[o.eng].get(name, 0) >= val:
                    continue
                waited[o.eng][name] = val
                w.append((name, val))
            o.waits = w
        self.sems, self.counts = sems, counts

    def emit(self, block):
        sems = self.sems
        by = {e: [o for o in self.ops if (not o.bar) and o.eng == e] for e in ENGS}
        final = [(n, c) for n, c in self.counts.items() if n.startswith("d_out")]

        def run(h, lst, fin=None):
            for o in lst:
                for name, val in o.waits:
                    h.wait_ge(sems[name], val)
                ins = o.fn(h)
                if o.inc:
                    ins.then_inc(sems[o.tok[0]], 16 if o.dma_key is not None else 1)
            if fin:
                for name, val in fin:
                    h.wait_ge(sems[name], val)

        @block.tensor
        def _(h):
            run(h, by["pe"])

        @block.scalar
        def _(h):
            run(h, by["act"])

        @block.vector
        def _(h):
            run(h, by["dve"])

        @block.gpsimd
        def _(h):
            run(h, by["pool"])

        @block.sync
        def _(h):
            run(h, by["sp"], final)


class Arena:
    BASE = 16640
    TOP = 228352

    def __init__(self, nc):
        self.nc = nc
        self.off = self.BASE
        self.n = 0
        self.peak = self.off
        self.top = self.TOP

    def alloc_top(self, name, shape, dt):
        esz = 4 if dt in (F32, I32) else 2
        nbytes = (int(np.prod(shape[1:])) * esz + 63) // 64 * 64
        self.top -= nbytes
        assert self.top >= self.off, (name, self.top, self.off)
        self.n += 1
        return self.nc.alloc_sbuf_tensor_at("%s_%d" % (name, self.n), list(shape), dt, offset=self.top)

    def release_top(self):
        self.top = self.TOP

    def alloc(self, name, shape, dt):
        esz = 4 if dt in (F32, I32) else 2
        nbytes = int(np.prod(shape[1:])) * esz
        nbytes = (nbytes + 63) // 64 * 64
        assert self.off + nbytes <= self.top, (name, self.off, nbytes, self.top)
        self.n += 1
        t = self.nc.alloc_sbuf_tensor_at("%s_%d" % (name, self.n), list(shape), dt, offset=self.off)
        self.off += nbytes
        self.peak = max(self.peak, self.off)
        return t

    def mark(self):
        return self.off

    def release(self, m):
        self.off = m


def build_nc(stop_after=None, dbg=()):
    nc = bass.Bass("TRN2", target_bir_lowering=False)
    dram_in = lambda name, shape, dt=F32: nc.dram_tensor(name, list(shape), dt, kind="ExternalInput").ap()
    xT_d = dram_in("xT", [D, S_])
    cT_d = dram_in("cT", [128, 8])
    pos_d = dram_in("pos", [1, S_], I32)
    w_ada_d = dram_in("w_ada", [D, 6 * D])
    bcol_d = dram_in("b_col", [128, 48])
    g1_d = dram_in("g1_col", [128, 8])
    g2_d = dram_in("g2_col", [128, 8])
    w_in_d = dram_in("w_in", [D, IN_COLS])
    gnaq_d = dram_in("gnaq_col", [128, 1])
    gnak_d = dram_in("gnak_col", [128, 1])
    tab_d = dram_in("tab", [128, 8, 24, 64])
    gql_d = dram_in("gql_col", [128, 2])
    gkvl_d = dram_in("gkvl_col", [128, 1])
    w_uq_d = dram_in("w_uq", [256, 768])
    w_ukv_d = dram_in("w_ukv", [128, 1024])
    gmq_d = dram_in("gmq_col", [96, 1])
    gmk_d = dram_in("gmk_col", [96, 1])
    ifr_d = dram_in("ifr_col", [96, 1])
    w_pn_d = dram_in("w_proj_na", [512, D])
    w_pm_d = dram_in("w_proj_mla", [512, D])
    w_out_d = dram_in("w_out", [D, D])
    w_r_d = dram_in("w_router", [D, E])
    ebias_d = dram_in("e_bias", [1, E])
    w_eg_d = dram_in("w_exp_gate", [E, D, 256])
    w_eu_d = dram_in("w_exp_up", [E, D, 256])
    w_ed_d = dram_in("w_exp_down", [E, 256, D])
    w_sg_d = dram_in("w_sh_gate", [D, 256])
    w_su_d = dram_in("w_sh_up", [D, 256])
    w_sd_d = dram_in("w_sh_down", [256, D])
    outT_d = nc.dram_tensor("outT", [D, S_], F32, kind="ExternalOutput").ap()
    dbg_out = {}

    es = ExitStack()
    with es:
        A = Arena(nc)
        S = Sched(nc)
        pall = es.enter_context(nc.psum_tensor("pall", [128, 8, 512], F32))
        pb = [pall[:, i, :] for i in range(8)]
        PB = ["pb%d" % i for i in range(8)]
        kc = lambda d_ap: d_ap.rearrange("(k p) n -> p k n", p=128)

        ident = A.alloc("ident", [128, 128], BF16)
        identf = A.alloc("identf", [128, 128], F32)
        ones = A.alloc("ones", [128, 128], BF16)
        blk = A.alloc("blk", [128, 128], BF16)
        one1 = A.alloc("one1", [1, 16], F32)
        modc = A.alloc("modc", [128, 48], F32)
        A1 = A.alloc("A1", [128, 8], F32)
        A2 = A.alloc("A2", [128, 8], F32)
        cols = A.alloc("cols", [128, 32], F32)
        epsc = A.alloc("epsc", [128, 1], F32)
        GQ, GK, GQL0, GKVL, GMQ, GMK, IFR = 0, 1, 2, 4, 5, 6, 7
        G1C, G2C = 8, 16


        wqkv = A.alloc_top("wqkv", [128, 8, 1536], BF16)
        tab = A.alloc_top("tab", [128, 8, 24, 64], BF16)

        mA = A.mark()
        cT = A.alloc("cT", [128, 8], F32)
        sc = A.alloc("sc", [128, 8], F32)
        bcol = A.alloc("bcol", [128, 48], F32)
        modrow = A.alloc("modrow", [1, 6 * D], F32)
        wada = [A.alloc("wada%d" % i, [128, 8, 512], BF16) for i in range(4)]
        scb = A.alloc("scb", [128, 8], BF16)
        for n in range(4):
            S.op("pool", lambda e, n=n: e.dma_start(out=wada[n][:], in_=kc(w_ada_d[:, n * 512:(n + 1) * 512])),
                 writes=["wada%d" % n], dma_key="wada%d" % n)
        S.op("pool", lambda e: e.memset(ident[:], 0.0), writes=["ident"])
        S.op("pool", lambda e: e.affine_select(out=ident[:], in_=ident[:], pattern=[[-1, 128]], compare_op=ALU.not_equal,
                                               fill=1.0, base=0, channel_multiplier=1), reads=["ident"], writes=["ident"])
        S.op("pool", lambda e: e.memset(identf[:], 0.0), writes=["identf"])
        S.op("pool", lambda e: e.affine_select(out=identf[:], in_=identf[:], pattern=[[-1, 128]], compare_op=ALU.not_equal,
                                               fill=1.0, base=0, channel_multiplier=1), reads=["identf"], writes=["identf"])
        S.op("pool", lambda e: e.memset(ones[:], 1.0), writes=["ones"])
        S.op("pool", lambda e: e.memset(blk[:], 0.0), writes=["blk"])
        S.op("pool", lambda e: e.memset(blk[0:64, 0:64], 1.0), reads=["blk"], writes=["blk"])
        S.op("pool", lambda e: e.memset(blk[64:128, 64:128], 1.0), reads=["blk"], writes=["blk"])
        S.op("pool", lambda e: e.memset(one1[:], 1.0), writes=["one1"])
        S.op("pool", lambda e: e.memset(epsc[:], EPS), writes=["epsc"])
        S.op("pool", lambda e: e.memset(cols[:], 0.0), writes=["cols"])
        small = [(gnaq_d, GQ, 128, 1), (gnak_d, GK, 128, 1), (gql_d, GQL0, 128, 2), (gkvl_d, GKVL, 128, 1),
                 (gmq_d, GMQ, 96, 1), (gmk_d, GMK, 96, 1), (ifr_d, IFR, 96, 1), (g1_d, G1C, 128, 8), (g2_d, G2C, 128, 8)]
        for i, (src, c0, npart, w) in enumerate(small):
            S.op("sp", lambda e, src=src, c0=c0, npart=npart, w=w: e.dma_start(out=cols[0:npart, c0:c0 + w], in_=src),
                 reads=["cols"], writes=["cols%d" % i], dma_key="cols")
        COLS = ["cols%d" % i for i in range(len(small))]
        S.op("dve", lambda e: e.tensor_scalar(out=cols[:, GQ:GQ + 1], in0=cols[:, GQ:GQ + 1], scalar1=64 ** -0.5, scalar2=None, op0=ALU.mult),
             reads=COLS, writes=["colsx"])
        S.op("dve", lambda e: e.tensor_scalar(out=cols[0:96, GMQ:GMQ + 1], in0=cols[0:96, GMQ:GMQ + 1], scalar1=96 ** -0.5, scalar2=None, op0=ALU.mult),
             reads=COLS + ["colsx"], writes=["colsy"])
        COLS = COLS + ["colsx", "colsy"]
        S.op("sp", lambda e: e.dma_start(out=cT[:], in_=cT_d), writes=["cT"], dma_key="cT")
        S.op("sp", lambda e: e.dma_start(out=bcol[:], in_=bcol_d), writes=["bcol"], dma_key="bcol")
        S.op("act", lambda e: e.activation(out=scb[:], in_=cT[:], func=AF.Silu), reads=["cT"], writes=["sc"])
        def mod_transpose(n):
            def fn(e, n=n):
                ins = None
                for j in range(4 * n, 4 * n + 4):
                    ins = e.matmul(pb[2][:, j:j + 1], lhsT=modrow[0:1, j * 128:(j + 1) * 128], rhs=one1[0:1, 0:1], start=True, stop=True)
                return ins
            S.op("pe", fn, reads=["modrow%d" % n, "one1"], writes=[PB[2]])

        for n in range(12):
            wb = wada[n % 4]
            if n >= 4:
                S.op("pool", lambda e, wb=wb, n=n: e.dma_start(out=wb[:], in_=kc(w_ada_d[:, n * 512:(n + 1) * 512])),
                     writes=["wada%d" % (n % 4)], dma_key="wada%d" % (n % 4))

            def fn(e, wb=wb, n=n):
                ins = None
                for k in range(8):
                    ins = e.matmul(pb[n % 2][0:1, :], lhsT=scb[:, k:k + 1], rhs=wb[:, k, :], start=(k == 0), stop=(k == 7))
                return ins
            S.op("pe", fn, reads=["sc", "wada%d" % (n % 4)], writes=[PB[n % 2]])
            S.op("act", lambda e, n=n: e.copy(out=modrow[0:1, n * 512:(n + 1) * 512], in_=pb[n % 2][0:1, :]),
                 reads=[PB[n % 2]], writes=["modrow%d" % n])
            if n >= 1:
                mod_transpose(n - 1)
        mod_transpose(11)
        S.op("dve", lambda e: e.tensor_tensor(out=modc[:], in0=pb[2][:, 0:48], in1=bcol[:], op=ALU.add), reads=[PB[2], "bcol"], writes=["modc"])
        S.op("dve", lambda e: e.scalar_tensor_tensor(out=A1[:], in0=modc[:, 8:16], scalar=1.0, in1=cols[:, G1C:G1C + 8], op0=ALU.add, op1=ALU.mult),
             reads=["modc"] + COLS, writes=["A1"])
        S.op("dve", lambda e: e.scalar_tensor_tensor(out=A2[:], in0=modc[:, 32:40], scalar=1.0, in1=cols[:, G2C:G2C + 8], op0=ALU.add, op1=ALU.mult),
             reads=["modc"] + COLS, writes=["A2"])
        SH1, GT1, SH2, GT2 = 0, 16, 24, 40
        if "modc" in dbg:
            dbg_out["modc"] = (modc, [128, 48])
        S.barrier()
        A.release(mA)
        if stop_after == "A":
            return _finish(nc, es, S, A, dbg_out, outT_d)

        r1 = A.mark()
        xT = A.alloc("xT", [128, 8, S_], F32)
        hT = nc.alloc_sbuf_tensor_at("hT_al", [128, 8, S_], BF16, offset=r1)
        yT_na = nc.alloc_sbuf_tensor_at("yTna_al", [128, 4, S_], BF16, offset=r1 + 32768)
        yT_mla = nc.alloc_sbuf_tensor_at("yTmla_al", [128, 4, S_], BF16, offset=r1 + 49152)

        def rinv_from(ss_ap, rs_ap, rinv_ap, n_feat, reads, wname, np_=128):
            S.op("act", lambda e: e.activation(out=rs_ap, in_=ss_ap, func=AF.Ln, bias=epsc[0:np_, :], scale=1.0 / n_feat),
                 reads=reads + ["epsc"], writes=[wname + "_rs"])
            S.op("act", lambda e: e.activation(out=rinv_ap, in_=rs_ap, func=AF.Exp, scale=-0.5), reads=[wname + "_rs"], writes=[wname])

        mB = A.mark()
        xch = [A.alloc("xch%d" % i, [128, 8, 512], F32) for i in range(2)]
        sqb = [A.alloc("sqb%d" % i, [128, 512], BF16) for i in range(2)]
        rs = [A.alloc("rs%d" % i, [128, 512], F32) for i in range(2)]
        rinv = [A.alloc("rinv%d" % i, [128, 512], F32) for i in range(2)]
        tmp = [A.alloc("tmp%d" % i, [128, 512], F32) for i in range(2)]
        b1_units = []
        for c in range(NC4):
            S.begin_unit()
            xb = xch[c % 2]
            cs = slice(c * 512, (c + 1) * 512)
            for k in range(8):
                S.op("sp", lambda e, xb=xb, k=k, cs=cs: e.dma_start(out=xb[:, k, :], in_=xT_d[k * 128:(k + 1) * 128, cs]),
                     writes=["xch%d_%d" % (c % 2, k)], dma_key="xch%d_%d" % (c % 2, k))
            for k in range(8):
                S.op("act", lambda e, xb=xb, k=k: e.activation(out=sqb[k % 2][:], in_=xb[:, k, :], func=AF.Square),
                     reads=["xch%d_%d" % (c % 2, k)], writes=["sqb%d" % (k % 2)])
                S.op("pe", lambda e, k=k, c=c: e.matmul(pb[c % 2][:], lhsT=ones[:], rhs=sqb[k % 2][:], start=(k == 0), stop=(k == 7)),
                     reads=["sqb%d" % (k % 2), "ones"], writes=[PB[c % 2]])
            rinv_from(pb[c % 2][:], rs[c % 2][:], rinv[c % 2][:], D, [PB[c % 2]], "rinv%d" % (c % 2))
            for k in range(8):
                S.op("dve", lambda e, xb=xb, k=k, c=c: e.scalar_tensor_tensor(out=tmp[k % 2][:], in0=xb[:, k, :], scalar=A1[:, k:k + 1],
                                                                              in1=rinv[c % 2][:], op0=ALU.mult, op1=ALU.mult),
                     reads=["xch%d_%d" % (c % 2, k), "A1", "rinv%d" % (c % 2)], writes=["tmp%d" % (k % 2)])
                S.op("act", lambda e, k=k, cs=cs: e.activation(out=hT[:, k, cs], in_=tmp[k % 2][:], func=AF.Identity,
                                                                bias=modc[:, SH1 + k:SH1 + k + 1], scale=1.0),
                     reads=["tmp%d" % (k % 2), "modc"], writes=["hT%d_%d" % (c, k)])
            b1_units.append(S.end_unit())
        S.emit_units(b1_units)
        XLAST = ["xch%d_%d" % (c2_, k) for c2_ in range(2) for k in range(8)]
        for g in range(3):
            S.op("pool", lambda e, g=g: e.dma_start(out=wqkv[:, :, g * 512:(g + 1) * 512], in_=kc(w_in_d[:, g * 512:(g + 1) * 512])),
                 reads=XLAST, writes=["wqkv%d" % g], dma_key="wqkv%d" % g)
        S.op("pool", lambda e: e.dma_start(out=tab[:], in_=tab_d), reads=XLAST, writes=["tab0"], dma_key="tab")
        HT = lambda c: ["hT%d_%d" % (c, k) for k in range(8)]
        if "hT" in dbg:
            dbg_out["hT"] = (hT, [128, 8, S_])
        S.barrier(skip=("wqkv0", "wqkv1", "wqkv2", "tab"))
        A.release(mB)
        if stop_after == "B1":
            return _finish(nc, es, S, A, dbg_out, outT_d)

        mNA = A.mark()
        qT_na = A.alloc("qz_na", [128, 8, S_], BF16)
        kT_na = A.alloc("kT_na", [128, 4, S_], BF16)
        v_na = A.alloc("v_na", [128, NT, 8, 65], BF16)
        S.op("pool", lambda e: e.memset(qT_na[:], 0.0), writes=["qz_zero"])
        sqn = [A.alloc("sqn%d" % i, [128, 512], BF16) for i in range(3)]
        rsn = [A.alloc("rsn%d" % i, [128, 512], F32) for i in range(3)]
        rinvn = [A.alloc("rinvn%d" % i, [128, 512], F32) for i in range(3)]
        S.op("pool", lambda e: e.memset(v_na[:, :, :, 64:65], 1.0), writes=["v_na_ones"])
        ui = 0
        na_units = []
        for which, dst, gcol in ((0, qT_na, GQ), (1, kT_na, GK)):
            for p in range(4):
                for c in range(NC4):
                    S.begin_unit()
                    cs = slice(c * 512, (c + 1) * 512)
                    bb = (0, 2, 6)[ui % 3]
                    b0, b1 = pb[bb], pb[bb + 1]
                    n0, n1 = PB[bb], PB[bb + 1]
                    col0 = which * 512 + p * 128

                    def fn(e, b0=b0, col0=col0, cs=cs):
                        ins = None
                        for k in range(8):
                            ins = e.matmul(b0[:], lhsT=wqkv[:, k, col0:col0 + 128], rhs=hT[:, k, cs], start=(k == 0), stop=(k == 7))
                        return ins
                    S.op("pe", fn, reads=["wqkv%d" % which] + HT(c), writes=[n0])
                    u2 = ui % 3
                    S.op("act", lambda e, b0=b0, u2=u2: e.activation(out=sqn[u2][:], in_=b0[:], func=AF.Square), reads=[n0], writes=["sqn%d" % u2])
                    S.op("pe", lambda e, b1=b1, u2=u2: e.matmul(b1[:], lhsT=blk[:], rhs=sqn[u2][:], start=True, stop=True),
                         reads=["sqn%d" % u2, "blk"], writes=[n1])
                    rinv_from(b1[:], rsn[u2][:], rinvn[u2][:], 64, [n1], "rinvn%d" % u2)
                    if which == 1:
                        S.op("dve", lambda e, b0=b0, u2=u2, dst=dst, p=p, cs=cs, gcol=gcol: e.scalar_tensor_tensor(
                            out=dst[:, p, cs], in0=b0[:], scalar=cols[:, gcol:gcol + 1], in1=rinvn[u2][:], op0=ALU.mult, op1=ALU.mult),
                            reads=[n0, "rinvn%d" % u2] + COLS, writes=["qk_%d_%d_%d" % (which, p, c)])
                    else:
                        for par in range(2):
                            rr = slice(par * 64, (par + 1) * 64)
                            S.op("dve", lambda e, b0=b0, u2=u2, dst=dst, p=p, cs=cs, gcol=gcol, rr=rr, par=par: e.scalar_tensor_tensor(
                                out=dst[rr, 2 * p + par, cs], in0=b0[rr, :], scalar=cols[rr, gcol:gcol + 1], in1=rinvn[u2][rr, :], op0=ALU.mult, op1=ALU.mult),
                                reads=[n0, "rinvn%d" % u2, "qz_zero"] + COLS, writes=["qk_%d_%d_%d_%d" % (which, p, c, par)])
                    ui += 1
                    na_units.append(S.end_unit())
        v_units = []
        for t in range(NT):
            S.begin_unit()
            b = pb[4 + t % 2]

            def fn(e, b=b, t=t):
                ins = None
                for k in range(8):
                    ins = e.matmul(b[:], lhsT=hT[:, k, t * 128:(t + 1) * 128], rhs=wqkv[:, k, 1024:1536], start=(k == 0), stop=(k == 7))
                return ins
            S.op("pe", fn, reads=["wqkv2"] + HT(t // 4), writes=[PB[4 + t % 2]])
            S.op("act", lambda e, b=b, t=t: e.copy(out=v_na[:, t, :, 0:64], in_=b[:].rearrange("p (h d) -> p h d", d=64)),
                 reads=[PB[4 + t % 2]], writes=["v_na%d" % t])
            v_units.append(S.end_unit())
        mixed = []
        for i, u in enumerate(na_units):
            mixed.append(u)
        S.emit_units(na_units, shift=(max(len(u) for u in na_units) + 2) // 3, extra=v_units)
        if "qT_na" in dbg:
            dbg_out["qT_na"] = (qT_na, [128, 8, S_])
            dbg_out["kT_na"] = (kT_na, [128, 4, S_])
            dbg_out["v_na"] = (v_na, [128, NT, 8, 65])
        S.barrier()
        if stop_after == "NAprep":
            return _finish(nc, es, S, A, dbg_out, outT_d)
        pT = [A.alloc("pT%d" % i, [128, 640], BF16) for i in range(2)]
        rden = [A.alloc("rden%d" % i, [128, 8], F32) for i in range(2)]
        ytok = [A.alloc("ytok%d" % i, [128, 8, 64], BF16) for i in range(2)]
        units = []
        for t in range(NT):
            if 2 <= t <= 13:
                js = [t + 2 - s for s in range(5)]
                ib = 0 + 0
                tbase, idx0 = 0, 0
            else:
                jmax = 3 if t < 2 else 15
                js = [jmax - s for s in range(4)]
                u0 = 8 - 2 * (jmax - t)
                tbase, idx0 = 10, u0 - 2
            for h in range(8):
                units.append((t, h, js, tbase + idx0))
        o_ps = [pb[4], pb[5]]
        O_PS = [PB[4], PB[5]]

        def emit_qk(i):
            t, h, js, ti0 = units[i]
            p, par = h // 2, h % 2
            r0 = par * 64
            sA, sB = pb[(i % 2) * 2], pb[(i % 2) * 2 + 1]

            def fn(e):
                ins = None
                for s, j in enumerate(js):
                    dst = sA[:, s * 128:(s + 1) * 128] if s < 4 else sB[:, 0:128]
                    e.matmul(dst, lhsT=kT_na[:, p, j * 128:(j + 1) * 128], rhs=qT_na[:, h, t * 128:(t + 1) * 128],
                             start=True, stop=False)
                    ins = e.matmul(dst, lhsT=ident[:], rhs=tab[:, h, ti0 + 2 * s:ti0 + 2 * s + 2, :], start=False, stop=True)
                return ins
            S.op("pe", fn, reads=[], writes=[PB[(i % 2) * 2], PB[(i % 2) * 2 + 1]])
            nj = len(js)
            b0i = (i % 2) * 2
            if nj == 5:
                flat = pall[:, b0i:b0i + 2, :].rearrange("p b n -> p (b n)")
                S.op("act", lambda e: e.activation(out=pT[i % 2][:, 0:640], in_=flat[:, 0:640], func=AF.Exp),
                     reads=[PB[b0i], PB[b0i + 1]], writes=["pTa%d" % (i % 2), "pTb%d" % (i % 2)])
            else:
                S.op("act", lambda e: e.activation(out=pT[i % 2][:, 0:512], in_=sA[:], func=AF.Exp),
                     reads=[PB[b0i]], writes=["pTa%d" % (i % 2)])

        def emit_pv(i):
            t, h, js, ti0 = units[i]
            ob = o_ps[h // 4]
            hh = h % 4

            def fn(e):
                ins = None
                for s, j in enumerate(js):
                    ins = e.matmul(ob[:, hh * 65:(hh + 1) * 65], lhsT=pT[i % 2][:, s * 128:(s + 1) * 128], rhs=v_na[:, j, h, :],
                                   start=(s == 0), stop=(s == len(js) - 1))
                return ins
            S.op("pe", fn, reads=["pTa%d" % (i % 2), "pTb%d" % (i % 2)], writes=[O_PS[h // 4]])
            if h == 7:
                t2 = t % 2
                for half in range(2):
                    ov = o_ps[half][:, 0:260].rearrange("p (h d) -> p h d", d=65)
                    S.op("dve", lambda e, ov=ov, half=half, t2=t2: e.reciprocal(out=rden[t2][:, half * 4:(half + 1) * 4], in_=ov[:, :, 64]),
                         reads=[O_PS[half]], writes=["rden%d_%d" % (t2, half)])
                    S.op("dve", lambda e, ov=ov, half=half, t2=t2: e.tensor_tensor(
                        out=ytok[t2][:, half * 4:(half + 1) * 4, :], in0=ov[:, :, 0:64],
                        in1=rden[t2][:, half * 4:(half + 1) * 4].unsqueeze(2).to_broadcast([128, 4, 64]), op=ALU.mult),
                        reads=[O_PS[half], "rden%d_%d" % (t2, half)], writes=["ytok%d_%d" % (t2, half)])
                trb = pb[6][:].bitcast(BF16)

                def fnt(e, t2=t2):
                    ins = None
                    yv = ytok[t2][:].rearrange("p h d -> p (h d)")
                    for bq in range(4):
                        ins = e.transpose(out=trb[:, bq * 128:(bq + 1) * 128], in_=yv[:, bq * 128:(bq + 1) * 128], identity=ident[:])
                    return ins
                S.op("pe", fnt, reads=["ytok%d_0" % t2, "ytok%d_1" % t2], writes=[PB[6]])
                S.op("act", lambda e, t=t: e.copy(out=yT_na[:, :, t * 128:(t + 1) * 128], in_=trb[:, 0:512].rearrange("p (b n) -> p b n", n=128)),
                     reads=[PB[6]], writes=["yT_na%d" % t])

        nu = len(units)
        emit_qk(0)
        for i in range(nu):
            if i + 1 < nu:
                emit_qk(i + 1)
            emit_pv(i)
        if "yT_na" in dbg:
            dbg_out["yT_na"] = (yT_na, [128, 4, S_])
        S.barrier()
        A.release(mNA)
        A.release_top()
        if stop_after == "NA":
            return _finish(nc, es, S, A, dbg_out, outT_d)

        mM = A.mark()
        qmT = A.alloc("qmT", [96, 8, S_], BF16)
        kmT = A.alloc("kmT", [96, 8, S_], BF16)
        kvlatT = A.alloc("kvlatT", [128, S_], BF16)
        wukv = A.alloc("wukv", [128, 1024], BF16)
        mMp = A.mark()
        qlatT = A.alloc("qlatT", [128, 2, S_], BF16)
        Ct = A.alloc("Ct", [96, S_], F32)
        Sn = A.alloc("Sn", [96, S_], F32)
        krope = A.alloc("krope", [96, S_], F32)
        mM1 = A.mark()
        wg3 = A.alloc("wg3", [128, 8, 416], BF16)
        wkr = A.alloc("wkr", [128, 8, 96], BF16)
        wkrr = A.alloc("wkrr", [128, 8, 96], BF16)
        posi = A.alloc("posi", [96, 512], I32)
        rndi = A.alloc("rndi", [96, 512], I32)
        ang = A.alloc("ang", [96, 512], F32)
        angf = A.alloc("angf", [96, 512], F32)
        sq96 = [A.alloc("sq96_%d" % i, [128, 512], BF16) for i in range(2)]
        sq96b = [A.alloc("sq96b_%d" % i, [128, 512], BF16) for i in range(2)]
        rs96 = [A.alloc("rs96_%d" % i, [128, 512], F32) for i in range(2)]
        rinv96 = [A.alloc("rinv96_%d" % i, [128, 512], F32) for i in range(2)]
        qraw = [A.alloc("qraw%d" % i, [96, 512], F32) for i in range(2)]
        tmp2 = [A.alloc("tmp2_%d" % i, [96, 512], F32) for i in range(2)]
        S.op("pool", lambda e: e.dma_start(out=wg3[:], in_=kc(w_in_d[:, C_QL:C_GN])), writes=["wg3"], dma_key="wg3")
        S.op("pool", lambda e: e.dma_start(out=wukv[:], in_=w_ukv_d), writes=["wukv"], dma_key="wukv")
        TWO_PI = 6.2831
        for c in range(NC4):
            cs = slice(c * 512, (c + 1) * 512)
            S.op("sp", lambda e, cs=cs: e.dma_start(out=posi[:], in_=pos_d[:, cs].to_broadcast([96, 512])), writes=["posi"], dma_key="posi")
            S.op("dve", lambda e: e.tensor_copy(out=angf[:], in_=posi[:]), reads=["posi"], writes=["angf"])
            S.op("dve", lambda e: e.tensor_scalar(out=ang[:], in0=angf[:], scalar1=cols[0:96, IFR:IFR + 1], scalar2=None, op0=ALU.mult),
                 reads=["angf"] + COLS, writes=["ang"])
            for which, dst, shift in ((0, Sn, 0.0), (1, Ct, 0.25)):
                if shift != 0.0:
                    S.op("dve", lambda e, shift=shift: e.tensor_scalar(out=ang[:], in0=ang[:], scalar1=shift, scalar2=None, op0=ALU.add),
                         reads=["ang"], writes=["ang"])
                S.op("dve", lambda e: e.tensor_copy(out=rndi[:], in_=ang[:]), reads=["ang"], writes=["rndi"])
                S.op("dve", lambda e: e.tensor_copy(out=angf[:], in_=rndi[:]), reads=["rndi"], writes=["angf"])
                S.op("dve", lambda e: e.tensor_tensor(out=angf[:], in0=ang[:], in1=angf[:], op=ALU.subtract), reads=["ang", "angf"], writes=["angf"])
                S.op("act", lambda e, dst=dst, cs=cs: e.activation(out=dst[:, cs], in_=angf[:], func=AF.Sin, scale=TWO_PI), reads=["angf"],
                     writes=["Sn%d" % c if which == 0 else "Ct%d" % c])
        S.op("pool", lambda e: e.memset(wkr[:], 0.0), writes=["wkr"])
        S.op("pool", lambda e: e.memset(wkrr[:], 0.0), writes=["wkrr"])
        S.op("pool", lambda e: e.tensor_copy(out=wkr[:, :, 64:96], in_=wg3[:, :, 384:416]), reads=["wg3", "wkr"], writes=["wkr"])
        S.op("pool", lambda e: e.tensor_scalar(out=wkrr[:, :, 64:80], in0=wg3[:, :, 400:416], scalar1=-1.0, scalar2=None, op0=ALU.mult),
             reads=["wg3", "wkrr"], writes=["wkrr"])
        S.op("pool", lambda e: e.tensor_copy(out=wkrr[:, :, 80:96], in_=wg3[:, :, 384:400]), reads=["wg3", "wkrr"], writes=["wkrr"])
        lat_units = []
        for c in range(NC4):
            S.begin_unit()
            cs = slice(c * 512, (c + 1) * 512)
            c2 = c % 2
            B0, B1, B2, B3 = [pb[4 * c2 + i] for i in range(4)]
            N0, N1, N2, N3 = [PB[4 * c2 + i] for i in range(4)]

            def proj(bank, col0, ncol, w=None, cs=cs):
                def fn(e):
                    ins = None
                    for k in range(8):
                        lhs = wg3[:, k, col0:col0 + ncol] if w is None else w[:, k, :]
                        ins = e.matmul(bank[0:ncol, :], lhsT=lhs, rhs=hT[:, k, cs], start=(k == 0), stop=(k == 7))
                    return ins
                return fn
            S.op("pe", proj(B0, 0, 128), reads=["wg3"] + HT(c), writes=[N0])
            S.op("pe", proj(B1, 128, 128), reads=["wg3"] + HT(c), writes=[N1])
            S.op("act", lambda e, c2=c2, B0=B0: e.activation(out=sq96[c2][:], in_=B0[:], func=AF.Square), reads=[N0], writes=["sq96_%d" % c2])
            S.op("act", lambda e, c2=c2, B1=B1: e.activation(out=sq96b[c2][:], in_=B1[:], func=AF.Square), reads=[N1], writes=["sq96b_%d" % c2])

            def fn(e, c2=c2, B2=B2):
                e.matmul(B2[:], lhsT=ones[:], rhs=sq96[c2][:], start=True, stop=False)
                return e.matmul(B2[:], lhsT=ones[:], rhs=sq96b[c2][:], start=False, stop=True)
            S.op("pe", fn, reads=["sq96_%d" % c2, "sq96b_%d" % c2, "ones"], writes=[N2])
            rinv_from(B2[:], rs96[c2][:], rinv96[c2][:], 256, [N2], "rinv96_%d" % c2)
            for j, (Bj, Nj) in enumerate(((B0, N0), (B1, N1))):
                S.op("dve", lambda e, j=j, c2=c2, cs=cs, Bj=Bj: e.scalar_tensor_tensor(out=qlatT[:, j, cs], in0=Bj[:], scalar=cols[:, GQL0 + j:GQL0 + j + 1],
                                                                                         in1=rinv96[c2][:], op0=ALU.mult, op1=ALU.mult),
                     reads=[Nj, "rinv96_%d" % c2] + COLS, writes=["qlatT%d_%d" % (c, j)])
            S.op("pe", proj(B3, 256, 128), reads=["wg3"] + HT(c), writes=[N3])
            S.op("act", lambda e, c2=c2, B3=B3: e.activation(out=sq96[c2][:], in_=B3[:], func=AF.Square), reads=[N3], writes=["sq96_%d" % c2])
            S.op("pe", lambda e, c2=c2, B2=B2: e.matmul(B2[:], lhsT=ones[:], rhs=sq96[c2][:], start=True, stop=True),
                 reads=["sq96_%d" % c2, "ones"], writes=[N2])
            rinv_from(B2[:], rs96[c2][:], rinv96[c2][:], 128, [N2], "rinv96_%d" % c2)
            S.op("dve", lambda e, c2=c2, cs=cs, B3=B3: e.scalar_tensor_tensor(out=kvlatT[:, cs], in0=B3[:], scalar=cols[:, GKVL:GKVL + 1],
                                                                               in1=rinv96[c2][:], op0=ALU.mult, op1=ALU.mult),
                 reads=[N3, "rinv96_%d" % c2] + COLS, writes=["kvlatT%d" % c])
            S.op("pe", proj(B0, 0, 96, wkr), reads=["wkr"] + HT(c), writes=[N0])
            S.op("pe", proj(B1, 0, 96, wkrr), reads=["wkrr"] + HT(c), writes=[N1])
            S.op("dve", lambda e, c2=c2, cs=cs, B0=B0: e.tensor_tensor(out=qraw[c2][:], in0=B0[0:96, :], in1=Ct[:, cs], op=ALU.mult),
                 reads=[N0, "Ct%d" % c], writes=["qraw%d" % c2])
            S.op("dve", lambda e, c2=c2, cs=cs, B1=B1: e.tensor_tensor(out=tmp2[c2][:], in0=B1[0:96, :], in1=Sn[:, cs], op=ALU.mult),
                 reads=[N1, "Sn%d" % c], writes=["tmp2_%d" % c2])
            S.op("dve", lambda e, c2=c2, cs=cs: e.tensor_tensor(out=krope[:, cs], in0=qraw[c2][:], in1=tmp2[c2][:], op=ALU.add),
                 reads=["qraw%d" % c2, "tmp2_%d" % c2], writes=["krope%d" % c])
            lat_units.append(S.end_unit())
        S.emit_units(lat_units)
        S.barrier()
        A.release(mM1)
        wuq = A.alloc("wuq", [128, 2, 768], BF16)
        wuqr = A.alloc("wuqr", [128, 2, 768], BF16)
        sqU = [A.alloc("sqU_%d" % i, [128, 2, 512], BF16) for i in range(2)]
        rsU = [A.alloc("rsU_%d" % i, [128, 2, 512], F32) for i in range(2)]
        qrawU = [A.alloc("qrawU%d" % i, [96, 2, 512], F32) for i in range(2)]
        tmpU = [A.alloc("tmpU_%d" % i, [96, 2, 512], F32) for i in range(1)] * 2
        sqk = [A.alloc("sqk_%d" % i, [96, 2, 512], BF16) for i in range(2)]
        sqr = A.alloc("sqr", [96, S_], BF16)
        S.op("pool", lambda e: e.memset(sqk[0][:], 0.0), writes=["sqk0"])
        S.op("pool", lambda e: e.memset(sqk[1][:], 0.0), writes=["sqk1"])
        S.op("pool", lambda e: e.memset(sqr[:], 0.0), writes=["sqr"])
        S.op("act", lambda e: e.activation(out=sqr[64:96, :], in_=krope[64:96, :], func=AF.Square), reads=["sqr"], writes=["sqr"])
        S.op("pool", lambda e: e.dma_start(out=wuq[:], in_=kc(w_uq_d)), writes=["wuq"], dma_key="wuq")
        wq4 = wuq[:].rearrange("p j (h d) -> p j h d", d=96)
        wr4 = wuqr[:].rearrange("p j (h d) -> p j h d", d=96)
        S.op("pool", lambda e: e.memset(wuqr[:], 0.0), writes=["wuqr"])
        for j in range(2):
            S.op("pool", lambda e, j=j: e.tensor_scalar(out=wr4[:, j, :, 64:80], in0=wq4[:, j, :, 80:96], scalar1=-1.0, scalar2=None, op0=ALU.mult),
                 reads=["wuq", "wuqr"], writes=["wuqr"])
            S.op("pool", lambda e, j=j: e.tensor_copy(out=wr4[:, j, :, 80:96], in_=wq4[:, j, :, 64:80]), reads=["wuq", "wuqr"], writes=["wuqr"])
        ui = 0
        mla_units = []
        for c in range(NC4):
            cs = slice(c * 512, (c + 1) * 512)
            for hp in range(4):
                S.begin_unit()
                h0 = 2 * hp
                u2 = ui % 2
                base = u2 * 4
                nAB = [PB[base + i] for i in range(4)]
                bcC = Ct[:, cs].unsqueeze(1).to_broadcast([96, 2, 512])
                bcS = Sn[:, cs].unsqueeze(1).to_broadcast([96, 2, 512])

                def fnq(e, w, off, base=base, h0=h0, cs=cs):
                    ins = None
                    for hh in range(2):
                        h = h0 + hh
                        e.matmul(pall[0:96, base + off + hh, :], lhsT=w[:, 0, h * 96:(h + 1) * 96], rhs=qlatT[:, 0, cs], start=True, stop=False)
                        ins = e.matmul(pall[0:96, base + off + hh, :], lhsT=w[:, 1, h * 96:(h + 1) * 96], rhs=qlatT[:, 1, cs], start=False, stop=True)
                    return ins
                S.op("pe", lambda e, f=fnq: f(e, wuq, 0), reads=["wuq"], writes=nAB[0:2])
                S.op("pe", lambda e, f=fnq: f(e, wuqr, 2), reads=["wuqr"], writes=nAB[2:4])
                S.op("dve", lambda e, base=base, u2=u2, bcC=bcC: e.tensor_tensor(out=qrawU[u2][:], in0=pall[0:96, base:base + 2, :], in1=bcC, op=ALU.mult),
                     reads=nAB[0:2], writes=["qrawU%d" % u2])
                S.op("dve", lambda e, base=base, u2=u2, bcS=bcS: e.tensor_tensor(out=tmpU[u2][:], in0=pall[0:96, base + 2:base + 4, :], in1=bcS, op=ALU.mult),
                     reads=nAB[2:4], writes=["tmpU0"])
                S.op("dve", lambda e, u2=u2: e.tensor_tensor(out=qrawU[u2][:], in0=qrawU[u2][:], in1=tmpU[u2][:], op=ALU.add),
                     reads=["qrawU%d" % u2, "tmpU0"], writes=["qrawU%d" % u2])
                S.op("act", lambda e, u2=u2: e.activation(out=sqU[u2][0:96], in_=qrawU[u2][:], func=AF.Square),
                     reads=["qrawU%d" % u2], writes=["sqU%d" % u2])

                def fss(e, base=base, u2=u2):
                    ins = None
                    for hh in range(2):
                        ins = e.matmul(pall[0:96, base + hh, :], lhsT=ones[0:96, 0:96], rhs=sqU[u2][0:96, hh, :], start=True, stop=True)
                    return ins
                S.op("pe", fss, reads=["sqU%d" % u2], writes=nAB[0:2])
                S.op("act", lambda e, base=base, u2=u2: e.activation(out=rsU[u2][0:96], in_=pall[0:96, base:base + 2, :], func=AF.Ln,
                                                                      bias=epsc[0:96, :], scale=1.0 / 96), reads=nAB[0:2], writes=["rsU%d" % u2])
                S.op("act", lambda e, u2=u2: e.activation(out=rsU[u2][0:96], in_=rsU[u2][0:96], func=AF.Exp, scale=-0.5), reads=["rsU%d" % u2], writes=["rsU%d" % u2])
                S.op("dve", lambda e, u2=u2, h0=h0, cs=cs: e.scalar_tensor_tensor(out=qmT[:, h0:h0 + 2, cs], in0=qrawU[u2][:], scalar=cols[0:96, GMQ:GMQ + 1],
                                                                                   in1=rsU[u2][0:96], op0=ALU.mult, op1=ALU.mult),
                     reads=["qrawU%d" % u2, "rsU%d" % u2], writes=["qmT%d_%d" % (hp, c)])
                def fk(e, base=base, h0=h0, cs=cs):
                    ins = None
                    for hh in range(2):
                        h = h0 + hh
                        ins = e.matmul(pall[0:64, base + 2 + hh, :], lhsT=wukv[:, h * 128:h * 128 + 64], rhs=kvlatT[:, cs], start=True, stop=True)
                    return ins
                S.op("pe", fk, reads=["wukv"], writes=nAB[2:4])
                S.op("act", lambda e, base=base, u2=u2: e.activation(out=sqk[u2][0:64], in_=pall[0:64, base + 2:base + 4, :], func=AF.Square),
                     reads=nAB[2:4] + ["sqk%d" % u2], writes=["sqk%d" % u2])

                def fssk(e, base=base, u2=u2, cs=cs):
                    ins = None
                    for hh in range(2):
                        e.matmul(pall[0:96, base + hh, :], lhsT=ones[0:96, 0:96], rhs=sqk[u2][0:96, hh, :], start=True, stop=False)
                        ins = e.matmul(pall[0:96, base + hh, :], lhsT=ones[0:96, 0:96], rhs=sqr[0:96, cs], start=False, stop=True)
                    return ins
                S.op("pe", fssk, reads=["sqk%d" % u2, "sqr"], writes=nAB[0:2])
                S.op("act", lambda e, base=base, u2=u2: e.activation(out=rsU[u2][0:96], in_=pall[0:96, base:base + 2, :], func=AF.Ln,
                                                                      bias=epsc[0:96, :], scale=1.0 / 96), reads=nAB[0:2], writes=["rsU%d" % u2])
                S.op("act", lambda e, u2=u2: e.activation(out=rsU[u2][0:96], in_=rsU[u2][0:96], func=AF.Exp, scale=-0.5), reads=["rsU%d" % u2], writes=["rsU%d" % u2])
                S.op("dve", lambda e, base=base, u2=u2, h0=h0, cs=cs: e.scalar_tensor_tensor(out=kmT[0:64, h0:h0 + 2, cs], in0=pall[0:64, base + 2:base + 4, :],
                                                                                              scalar=cols[0:64, GMK:GMK + 1], in1=rsU[u2][0:64], op0=ALU.mult, op1=ALU.mult),
                     reads=nAB[2:4] + ["rsU%d" % u2], writes=["kmTa%d_%d" % (hp, c)])
                S.op("dve", lambda e, u2=u2, h0=h0, cs=cs: e.scalar_tensor_tensor(out=kmT[64:96, h0:h0 + 2, cs],
                                                                                   in0=krope[64:96, cs].unsqueeze(1).to_broadcast([32, 2, 512]),
                                                                                   scalar=cols[64:96, GMK:GMK + 1], in1=rsU[u2][64:96], op0=ALU.mult, op1=ALU.mult),
                     reads=["rsU%d" % u2], writes=["kmTb%d_%d" % (hp, c)])
                ui += 1
                mla_units.append(S.end_unit())
        S.emit_units(mla_units)
        if "qmT" in dbg:
            dbg_out["qmT"] = (qmT, [96, 8, S_])
            dbg_out["kmT"] = (kmT, [96, 8, S_])
        S.barrier()
        A.release(mMp)
        if stop_after == "MLAprep":
            return _finish(nc, es, S, A, dbg_out, outT_d)
        vm = A.alloc("vm", [128, NT, 8, 65], BF16)
        wgn = A.alloc_top("wgn", [128, 8, D], BF16)
        wgm = A.alloc_top("wgm", [128, 8, D], BF16)
        S.op("pool", lambda e: e.memset(vm[:, :, :, 64:65], 1.0), writes=["vm_ones"])
        wv3 = wukv[:].rearrange("p (h d) -> p h d", d=128)
        for t in range(NT):
            b = pb[3 + 4 * (t % 2)]
            S.op("pe", lambda e, b=b, t=t: e.matmul(b[:], lhsT=kvlatT[:, t * 128:(t + 1) * 128], rhs=wv3[:, :, 64:128], start=True, stop=True),
                 reads=["wukv"], writes=[PB[3 + 4 * (t % 2)]])
            S.op("act", lambda e, b=b, t=t: e.copy(out=vm[:, t, :, 0:64], in_=b[:].rearrange("p (h d) -> p h d", d=64)),
                 reads=[PB[3 + 4 * (t % 2)]], writes=["vm%d" % t])
        if "vm" in dbg:
            dbg_out["vm"] = (vm, [128, NT, 8, 65])
        S.op("pool", lambda e: e.dma_start(out=wgn[:], in_=kc(w_in_d[:, C_GN:C_GM])), writes=["wgn"], dma_key="wgn")
        S.op("pool", lambda e: e.dma_start(out=wgm[:], in_=kc(w_in_d[:, C_GM:IN_COLS])), writes=["wgm"], dma_key="wgm")
        pTm = [A.alloc("pTm%d" % i, [128, 512], BF16) for i in range(3)]
        rdm = [A.alloc("rdm%d" % i, [128, 4], F32) for i in range(2)]
        ytokM = A.alloc("ytokM", [128, NT, 512], BF16)
        steps = [(h, c, j) for h in range(8) for c in range(NC4) for j in range(NT)]

        def m_qk(i):
            h, c, j = steps[i]
            b3 = i % 3
            S.op("pe", lambda e: e.matmul(pb[b3][:], lhsT=kmT[:, h, j * 128:(j + 1) * 128], rhs=qmT[:, h, c * 512:(c + 1) * 512], start=True, stop=True),
                 reads=[], writes=[PB[b3]])
            S.op("act", lambda e: e.activation(out=pTm[b3][:], in_=pb[b3][:], func=AF.Exp), reads=[PB[b3]], writes=["pTm%d" % b3])

        def m_pv(i):
            h, c, j = steps[i]
            b3 = i % 3
            u = (h * NC4 + c) % 2
            ob = pb[4 + u]

            def fn(e):
                ins = None
                for qs in range(4):
                    ins = e.matmul(ob[:, qs * 65:(qs + 1) * 65], lhsT=pTm[b3][:, qs * 128:(qs + 1) * 128], rhs=vm[:, j, h, :],
                                   start=(j == 0 and qs == 0), stop=(j == NT - 1), skip_group_check=True)
                return ins
            S.op("pe", fn, reads=["pTm%d" % b3, "vm%d" % j, "vm_ones"], writes=[PB[4 + u]])
            if j == NT - 1:
                ov = ob[:, 0:260].rearrange("p (q d) -> p q d", d=65)
                S.op("dve", lambda e: e.reciprocal(out=rdm[u][:], in_=ov[:, :, 64]), reads=[PB[4 + u]], writes=["rdm%d" % u])
                S.op("dve", lambda e: e.tensor_tensor(out=ytokM[:, c * 4:(c + 1) * 4, h * 64:(h + 1) * 64], in0=ov[:, :, 0:64],
                                                      in1=rdm[u][:].unsqueeze(2).to_broadcast([128, 4, 64]), op=ALU.mult),
                     reads=[PB[4 + u], "rdm%d" % u], writes=["ytokM_%d_%d" % (h, c)])

        ns = len(steps)
        m_qk(0)
        m_qk(1)
        for i in range(ns):
            if i + 2 < ns:
                m_qk(i + 2)
            m_pv(i)
        trb = pb[6][:].bitcast(BF16)
        trb2 = pb[7][:].bitcast(BF16)
        for t in range(NT):
            tb = trb if t % 2 == 0 else trb2

            def fnt(e, t=t, tb=tb):
                ins = None
                for bq in range(4):
                    ins = e.transpose(out=tb[:, bq * 128:(bq + 1) * 128], in_=ytokM[:, t, bq * 128:(bq + 1) * 128], identity=ident[:])
                return ins
            S.op("pe", fnt, reads=["ytokM_%d_%d" % (h, t // 4) for h in range(8)], writes=[PB[6 + t % 2]])
            S.op("act", lambda e, t=t, tb=tb: e.copy(out=yT_mla[:, :, t * 128:(t + 1) * 128], in_=tb[:, 0:512].rearrange("p (b n) -> p b n", n=128)),
                 reads=[PB[6 + t % 2]], writes=["yT_mla%d" % t])
        if "yT_mla" in dbg:
            dbg_out["yT_mla"] = (yT_mla, [128, 4, S_])
        S.barrier()
        A.release(mM)
        if stop_after == "MLA":
            return _finish(nc, es, S, A, dbg_out, outT_d)

        mG = A.mark()
        mergedT = A.alloc("mergedT", [128, 8, S_], BF16)
        wout = A.alloc("wout", [128, 8, D], BF16)
        mG1 = A.mark()
        wpn = A.alloc("wpn", [128, 4, D], BF16)
        wpm = A.alloc("wpm", [128, 4, D], BF16)
        sgn = [A.alloc("sgn%d" % i, [128, 512], F32) for i in range(2)]
        sgm = [A.alloc("sgm%d" % i, [128, 512], F32) for i in range(2)]
        mm1 = [A.alloc("mm1_%d" % i, [128, 512], F32) for i in range(2)]
        mm2 = [A.alloc("mm2_%d" % i, [128, 512], F32) for i in range(2)]
        S.op("pool", lambda e: e.dma_start(out=wpn[:], in_=kc(w_pn_d)), writes=["wpn"], dma_key="wpn")
        S.op("pool", lambda e: e.dma_start(out=wpm[:], in_=kc(w_pm_d)), writes=["wpm"], dma_key="wpm")
        S.op("pool", lambda e: e.dma_start(out=wout[:], in_=kc(w_out_d)), writes=["wout"], dma_key="wout")
        ui = 0
        for c in range(NC4):
            cs = slice(c * 512, (c + 1) * 512)
            for j in range(8):
                u2 = ui % 2
                banks = [pb[u2 * 4 + i] for i in range(4)]
                names = [PB[u2 * 4 + i] for i in range(4)]
                js = slice(j * 128, (j + 1) * 128)

                def mk(bank, w, src, nk, js=js, cs=cs):
                    def fn(e):
                        ins = None
                        for k in range(nk):
                            ins = e.matmul(bank[:], lhsT=w[:, k, js], rhs=src[:, k, cs], start=(k == 0), stop=(k == nk - 1))
                        return ins
                    return fn
                S.op("pe", mk(banks[0], wgn, hT, 8), reads=["wgn"], writes=[names[0]])
                S.op("pe", mk(banks[1], wgm, hT, 8), reads=["wgm"], writes=[names[1]])
                S.op("pe", mk(banks[2], wpn, yT_na, 4), reads=["wpn"], writes=[names[2]])
                S.op("pe", mk(banks[3], wpm, yT_mla, 4), reads=["wpm"], writes=[names[3]])
                S.op("act", lambda e, u2=u2, b=banks[0]: e.activation(out=sgn[u2][:], in_=b[:], func=AF.Sigmoid), reads=[names[0]], writes=["sgn%d" % u2])
                S.op("act", lambda e, u2=u2, b=banks[1]: e.activation(out=sgm[u2][:], in_=b[:], func=AF.Sigmoid), reads=[names[1]], writes=["sgm%d" % u2])
                S.op("dve", lambda e, u2=u2, b=banks[2]: e.tensor_tensor(out=mm1[u2][:], in0=b[:], in1=sgn[u2][:], op=ALU.mult),
                     reads=[names[2], "sgn%d" % u2], writes=["mm1_%d" % u2])
                S.op("dve", lambda e, u2=u2, b=banks[3]: e.tensor_tensor(out=mm2[u2][:], in0=b[:], in1=sgm[u2][:], op=ALU.mult),
                     reads=[names[3], "sgm%d" % u2], writes=["mm2_%d" % u2])
                S.op("pool", lambda e, u2=u2, j=j, cs=cs: e.tensor_tensor(out=mergedT[:, j, cs], in0=mm1[u2][:], in1=mm2[u2][:], op=ALU.add),
                     reads=["mm1_%d" % u2, "mm2_%d" % u2], writes=["mergedT_%d_%d" % (j, c)])
                ui += 1
        if "mergedT" in dbg:
            dbg_out["mergedT"] = (mergedT, [128, 8, S_])
        S.barrier()
        A.release(mG1)
        A.release_top()
        if stop_after == "MERGE1":
            return _finish(nc, es, S, A, dbg_out, outT_d)
        for k in range(8):
            S.op("sp", lambda e, k=k: e.dma_start(out=xT[:, k, :], in_=xT_d[k * 128:(k + 1) * 128, :]), writes=["xT_%d" % k], dma_key="xTl%d" % k)
        ui = 0
        for c in range(NC4):
            cs = slice(c * 512, (c + 1) * 512)
            for dc_ in range(8):
                b = pb[ui % 4]

                def fn(e, b=b, dc_=dc_, cs=cs):
                    ins = None
                    for k in range(8):
                        ins = e.matmul(b[:], lhsT=wout[:, k, dc_ * 128:(dc_ + 1) * 128], rhs=mergedT[:, k, cs], start=(k == 0), stop=(k == 7))
                    return ins
                S.op("pe", fn, reads=["wout"], writes=[PB[ui % 4]])
                S.op("dve", lambda e, b=b, dc_=dc_, cs=cs: e.scalar_tensor_tensor(out=xT[:, dc_, cs], in0=b[:], scalar=modc[:, GT1 + dc_:GT1 + dc_ + 1],
                                                                                    in1=xT[:, dc_, cs], op0=ALU.mult, op1=ALU.add),
                     reads=[PB[ui % 4], "xT_%d" % dc_], writes=["x1_%d_%d" % (dc_, c)])
                ui += 1
        if "x1T" in dbg:
            dbg_out["x1T"] = (xT, [128, 8, S_])
        S.barrier()
        A.release(mG)
        if stop_after == "MERGE":
            return _finish(nc, es, S, A, dbg_out, outT_d)

        h2T = A.alloc("h2T", [128, 8, S_], BF16)
        gatesT = A.alloc("gatesT", [64, S_], BF16)
        gT_dram = nc.dram_tensor("gT_scratch", [E, S_], BF16, kind="Internal").ap()
        gbs = [A.alloc("gbs%d" % i, [128, S_], BF16) for i in range(4)]
        wgu = [A.alloc("wgu%d" % i, [128, 8, 512], BF16) for i in range(2)]
        wdn = [A.alloc("wdn%d" % i, [128, 2, D], BF16) for i in range(2)]
        aT = [[A.alloc("aT%d_%d" % (i, j), [128, 2, 512], BF16) for j in range(2)] for i in range(2)]
        sgs = [A.alloc("sgs%d" % i, [128, 512], BF16) for i in range(2)]
        tts = [A.alloc("tts%d" % i, [128, 512], BF16) for i in range(2)]
        GT_ALL = ["gatesT%d" % c for c in range(NC4)]

        pos_of = lambda e_: 0 if e_ == E else e_ + 1
        slot_of = lambda e_: pos_of(e_) % 4

        def load_expert(e_):
            sl = slot_of(e_)
            if e_ < E:
                srcs = (w_eg_d[e_], w_eu_d[e_], w_ed_d[e_])
            else:
                srcs = (w_sg_d, w_su_d, w_sd_d)
            rd = GT_ALL if pos_of(e_) in (2, 3) else []
            S.op("pool", lambda e: e.dma_start(out=wgu[sl][:, :, 0:256], in_=kc(srcs[0])), reads=rd, writes=["wgu%d" % sl], dma_key="wslg%d" % sl)
            S.op("pool", lambda e: e.dma_start(out=wgu[sl][:, :, 256:512], in_=kc(srcs[1])), reads=rd, writes=["wgu%d" % sl], dma_key="wslg%d" % sl)
            S.op("pool", lambda e: e.dma_start(out=wdn[sl][:], in_=kc(srcs[2])), reads=rd, writes=["wdn%d" % sl], dma_key="wsld%d" % sl)
        load_expert(E)
        load_expert(0)
        mP = A.mark()
        scores = A.alloc("scores", [128, NT, E], F32)
        wr = A.alloc("wr", [128, 8, E], F32)
        ebc = A.alloc("ebc", [128, E], F32)
        mP2 = A.mark()
        h2f = A.alloc("h2f", [128, 3, 512], F32)
        sqb2 = [A.alloc("sqb2_%d" % i, [128, 512], BF16) for i in range(2)]
        rs2 = [A.alloc("rs2_%d" % i, [128, 512], F32) for i in range(2)]
        rinv2 = [A.alloc("rinv2_%d" % i, [128, 512], F32) for i in range(2)]
        tmp3 = [A.alloc("tmp3_%d" % i, [128, 512], F32) for i in range(2)]
        S.op("sp", lambda e: e.dma_start(out=wr[:], in_=kc(w_r_d)), writes=["wr"], dma_key="wr")
        S.op("sp", lambda e: e.dma_start(out=ebc[:], in_=ebias_d.to_broadcast([128, E])), writes=["ebc"], dma_key="ebc")
        prep_units = []
        for c in range(NC4):
            S.begin_unit()
            cs = slice(c * 512, (c + 1) * 512)
            c2 = c % 2
            for k in range(8):
                S.op("act", lambda e, k=k, cs=cs: e.activation(out=sqb2[k % 2][:], in_=xT[:, k, cs], func=AF.Square), writes=["sqb2_%d" % (k % 2)])
                S.op("pe", lambda e, k=k, c2=c2: e.matmul(pb[c2][:], lhsT=ones[:], rhs=sqb2[k % 2][:], start=(k == 0), stop=(k == 7)),
                     reads=["sqb2_%d" % (k % 2)], writes=[PB[c2]])
            rinv_from(pb[c2][:], rs2[c2][:], rinv2[c2][:], D, [PB[c2]], "rinv2_%d" % c2)
            for k in range(8):
                kk = (c * 8 + k) % 3
                S.op("dve", lambda e, k=k, cs=cs, c2=c2: e.scalar_tensor_tensor(out=tmp3[k % 2][:], in0=xT[:, k, cs], scalar=A2[:, k:k + 1], in1=rinv2[c2][:],
                                                                                 op0=ALU.mult, op1=ALU.mult), reads=["rinv2_%d" % c2], writes=["tmp3_%d" % (k % 2)])
                S.op("act", lambda e, k=k, kk=kk: e.activation(out=h2f[:, kk, :], in_=tmp3[k % 2][:], func=AF.Identity, bias=modc[:, SH2 + k:SH2 + k + 1], scale=1.0),
                     reads=["tmp3_%d" % (k % 2)], writes=["h2f_%d" % kk])
                S.op("dve", lambda e, k=k, kk=kk, cs=cs: e.tensor_copy(out=h2T[:, k, cs], in_=h2f[:, kk, :]), reads=["h2f_%d" % kk], writes=["h2T_%d_%d" % (c, k)])

                def fr(e, k=k, kk=kk, c2=c2):
                    ins = None
                    for t4 in range(4):
                        ins = e.matmul(pb[2 + c2][:, t4 * E:(t4 + 1) * E], lhsT=h2f[:, kk, t4 * 128:(t4 + 1) * 128], rhs=wr[:, k, :],
                                       start=(k == 0 and t4 == 0), stop=(k == 7), skip_group_check=True)
                    return ins
                S.op("pe", fr, reads=["h2f_%d" % kk, "wr"], writes=[PB[2 + c2]])
            S.op("act", lambda e, c=c, c2=c2: e.activation(out=scores[:, c * 4:(c + 1) * 4, :], in_=pb[2 + c2][:, 0:256].rearrange("p (t x) -> p t x", x=E),
                                                    func=AF.Sigmoid), reads=[PB[2 + c2]], writes=["scores%d" % c])
            prep_units.append(S.end_unit())
        S.emit_units(prep_units)
        SC = ["scores%d" % c for c in range(NC4)]
        S.barrier()
        A.release(mP2)
        sel = A.alloc("sel", [128, NT, E], F32)
        eqm = A.alloc("bufA", [128, NT, E], F32)
        sel2 = A.alloc("bufB", [128, NT, E], F32)
        cmpb = A.alloc("cmpb", [128, NT, 8, 8], F32)
        selm = A.alloc("selm", [128, NT, E], F32)
        emask, wsel = eqm, sel2
        gates = nc.alloc_sbuf_tensor_at("gates_al", [128, NT, E], F32, offset=A.mark() - 2 * 4096)
        m1 = A.alloc("m1", [128, NT * 8], F32)
        m2 = A.alloc("m2", [128, NT * 8], F32)
        gs = A.alloc("gs", [128, NT * 8], F32)
        cnt = A.alloc("cnt", [128, NT * 8], F32)
        gmask = A.alloc("gmask", [128, NT * 8], F32)
        pen = A.alloc("pen", [128, NT * 8], F32)
        t8 = A.alloc("t8", [128, NT, 8], F32)
        wsum = A.alloc("wsum", [128, NT], F32)
        rw = A.alloc("rw", [128, NT], F32)
        v8 = lambda t_: t_[:].rearrange("p t (g x) -> p (t g) x", x=8)
        bc8 = lambda t_: t_[:].unsqueeze(2).to_broadcast([128, NT * 8, 8])
        S.op("dve", lambda e: e.tensor_tensor(out=sel[:], in0=scores[:], in1=ebc[:].unsqueeze(1).to_broadcast([128, NT, E]), op=ALU.add),
             reads=SC + ["ebc"], writes=["sel"])
        S.op("dve", lambda e: e.tensor_reduce(out=m1[:], in_=v8(sel), axis=AX.X, op=ALU.max), reads=["sel"], writes=["m1"])
        S.op("dve", lambda e: e.tensor_tensor(out=v8(eqm), in0=v8(sel), in1=bc8(m1), op=ALU.is_equal), reads=["sel", "m1"], writes=["bufA"])
        S.op("dve", lambda e: e.scalar_tensor_tensor(out=sel2[:], in0=eqm[:], scalar=-10.0, in1=sel[:], op0=ALU.mult, op1=ALU.add),
             reads=["bufA", "sel"], writes=["bufB"])
        S.op("dve", lambda e: e.tensor_reduce(out=m2[:], in_=v8(sel2), axis=AX.X, op=ALU.max), reads=["bufB"], writes=["m2"])
        S.op("dve", lambda e: e.tensor_tensor(out=gs[:], in0=m1[:], in1=m2[:], op=ALU.add), reads=["m1", "m2"], writes=["gs"])
        gs3 = gs[:].rearrange("p (t g) -> p t g", g=8)
        S.op("dve", lambda e: e.tensor_tensor(out=cmpb[:], in0=gs3.unsqueeze(2).to_broadcast([128, NT, 8, 8]),
                                              in1=gs3.unsqueeze(3).to_broadcast([128, NT, 8, 8]), op=ALU.is_gt), reads=["gs"], writes=["cmpb"])
        S.op("dve", lambda e: e.tensor_reduce(out=cnt[:], in_=cmpb[:].rearrange("p t g x -> p (t g) x"), axis=AX.X, op=ALU.add),
             reads=["cmpb"], writes=["cnt"])
        S.op("dve", lambda e: e.tensor_scalar(out=gmask[:], in0=cnt[:], scalar1=3.5, scalar2=None, op0=ALU.is_lt), reads=["cnt"], writes=["gmask"])
        S.op("dve", lambda e: e.tensor_scalar(out=pen[:], in0=gmask[:], scalar1=10.0, scalar2=-10.0, op0=ALU.mult, op1=ALU.add),
             reads=["gmask"], writes=["pen"])
        S.op("dve", lambda e: e.tensor_tensor(out=v8(selm), in0=v8(sel), in1=bc8(gmask), op=ALU.mult), reads=["sel", "gmask"], writes=["selm"])
        S.op("dve", lambda e: e.tensor_tensor(out=v8(selm), in0=v8(selm), in1=bc8(pen), op=ALU.add), reads=["selm", "pen"], writes=["selm"])
        for t in range(NT):
            S.op("dve", lambda e, t=t: e.max(out=t8[:, t, :], in_=selm[:, t, :]), reads=["selm"], writes=["t8_%d" % t])
        T8 = ["t8_%d" % t for t in range(NT)]
        S.op("dve", lambda e: e.tensor_tensor(out=emask[:], in0=selm[:], in1=t8[:, :, 7:8].to_broadcast([128, NT, E]), op=ALU.is_ge),
             reads=["selm"] + T8, writes=["bufA"])
        S.op("dve", lambda e: e.tensor_tensor(out=wsel[:], in0=scores[:], in1=emask[:], op=ALU.mult), reads=["bufA"], writes=["bufB"])
        S.op("dve", lambda e: e.tensor_reduce(out=wsum[:], in_=wsel[:], axis=AX.X, op=ALU.add), reads=["bufB"], writes=["wsum"])
        S.op("dve", lambda e: e.reciprocal(out=rw[:], in_=wsum[:]), reads=["wsum"], writes=["rw"])
        S.op("dve", lambda e: e.scalar_tensor_tensor(out=gates[:], in0=wsel[:], scalar=2.5, in1=rw[:].unsqueeze(2).to_broadcast([128, NT, E]),
                                                     op0=ALU.mult, op1=ALU.mult), reads=["bufB", "rw", "cnt"], writes=["gates", "cmpb"])
        def post_route_a():
            for c in range(NC4):
                def fng(e, c=c):
                    ins = None
                    for t4 in range(4):
                        ins = e.transpose(out=pb[4 + c % 2][0:E, t4 * 128:(t4 + 1) * 128], in_=gates[:, c * 4 + t4, :], identity=identf[:])
                    return ins
                S.op("pe", fng, reads=["gates", "identf"], writes=[PB[4 + c % 2]])
                S.op("act", lambda e, c=c: e.copy(out=gatesT[:, c * 512:(c + 1) * 512], in_=pb[4 + c % 2][0:E, :]), reads=[PB[4 + c % 2]],
                     writes=["gatesT%d" % c])
        if "gates" in dbg:
            dbg_out["gates"] = (gates, [128, NT, E])
            dbg_out["h2T"] = (h2T, [128, 8, S_])
            dbg_out["gatesT"] = (gatesT, [64, S_])
        def load_gates(e_):
            S.op("sp", lambda e: e.dma_start(out=gbs[e_ % 4][:], in_=gT_dram[e_:e_ + 1, :].to_broadcast([128, S_])), reads=["gT_dram"],
                 writes=["gbs%d" % (e_ % 4)], dma_key="gbs%d" % (e_ % 4))

        def post_route():
            post_route_a()
            S.op("sp", lambda e: e.dma_start(out=gT_dram, in_=gatesT[:]), reads=GT_ALL, writes=["gT_dram"], dma_key="gTd")
            for e_ in range(4):
                load_gates(e_)
            if len(wgu) == 4:
                load_expert(1)
                load_expert(2)
        if stop_after == "ROUTE":
            post_route()
            return _finish(nc, es, S, A, dbg_out, outT_d)

        A.release(mP2)
        wgu += [A.alloc("wgu%d" % i, [128, 8, 512], BF16) for i in range(2, 4)]
        wdn += [A.alloc("wdn%d" % i, [128, 2, D], BF16) for i in range(2, 4)]
        _ng = [d[3:] for d in dbg if d.startswith("ng=")]
        NG = int(_ng[0]) if _ng else 33
        groups = [[E]] + [[2 * g, 2 * g + 1] for g in range(32)]
        groups = groups[:NG]
        stepsE = [(g, c) for g in range(len(groups)) for c in range(NC4)]
        cnts = {"u": 0, "e": 0, "y": 0}

        def up_step(si):
            g, c = stepsE[si]
            cs = slice(c * 512, (c + 1) * 512)
            par = si % 2
            for el, e_ in enumerate(groups[g]):
                sl = slot_of(e_)
                if c == 0 and e_ + 4 <= E and (e_ + 4) // 2 < len(groups) + 0:
                    pass
                ei = cnts["e"]
                cnts["e"] += 1
                gb_bank = pb[4 + ei % 2]
                H2 = ["h2T_%d_%d" % (c, k) for k in range(8)]
                for fh in range(2):
                    ui = cnts["u"]
                    cnts["u"] += 1
                    u2 = ui % 2
                    bg, bu = pb[u2], pb[2 + u2]

                    def fn(e, bank, off, sl=sl, cs=cs):
                        ins = None
                        for k in range(8):
                            ins = e.matmul(bank[:], lhsT=wgu[sl][:, k, off:off + 128], rhs=h2T[:, k, cs], start=(k == 0), stop=(k == 7))
                        return ins
                    S.op("pe", lambda e, f=fn, bg=bg, fh=fh: f(e, bg, fh * 128), reads=["wgu%d" % sl] + H2, writes=[PB[u2]])
                    S.op("pe", lambda e, f=fn, bu=bu, fh=fh: f(e, bu, 256 + fh * 128), reads=["wgu%d" % sl] + H2, writes=[PB[2 + u2]])
                    S.op("act", lambda e, bg=bg, u2=u2: e.activation(out=sgs[u2][:], in_=bg[:], func=AF.Silu), reads=[PB[u2]], writes=["sgs%d" % u2])
                    if e_ < E:
                        S.op("dve", lambda e, bu=bu, u2=u2: e.tensor_tensor(out=tts[u2][:], in0=bu[:], in1=sgs[u2][:], op=ALU.mult),
                             reads=[PB[2 + u2], "sgs%d" % u2], writes=["tts%d" % u2])
                        S.op("dve", lambda e, u2=u2, e_=e_, par=par, el=el, fh=fh, cs=cs: e.tensor_tensor(out=aT[par][el][:, fh, :], in0=tts[u2][:],
                                                                                                          in1=gbs[e_ % 4][:, cs], op=ALU.mult),
                             reads=["tts%d" % u2, "gbs%d" % (e_ % 4)], writes=["aT%d_%d_%d" % (par, el, fh)])
                    else:
                        S.op("dve", lambda e, bu=bu, u2=u2, par=par, el=el, fh=fh: e.tensor_tensor(out=aT[par][el][:, fh, :], in0=bu[:], in1=sgs[u2][:], op=ALU.mult),
                             reads=[PB[2 + u2], "sgs%d" % u2], writes=["aT%d_%d_%d" % (par, el, fh)])

        def down_step(si):
            g, c = stepsE[si]
            cs = slice(c * 512, (c + 1) * 512)
            par = si % 2
            grp = groups[g]
            for dc_ in range(8):
                yi = cnts["y"]
                cnts["y"] += 1
                yb = pb[4 + yi % 4]

                def fn(e, yb=yb, dc_=dc_):
                    ins = None
                    n = len(grp) * 2
                    i = 0
                    for el, e_ in enumerate(grp):
                        for fh in range(2):
                            ins = e.matmul(yb[:], lhsT=wdn[slot_of(e_)][:, fh, dc_ * 128:(dc_ + 1) * 128], rhs=aT[par][el][:, fh, :], start=(i == 0), stop=(i == n - 1))
                            i += 1
                    return ins
                S.op("pe", fn, reads=["wdn%d" % slot_of(e_) for e_ in grp] + ["aT%d_%d_%d" % (par, el, fh) for el in range(len(grp)) for fh in range(2)],
                     writes=[PB[4 + yi % 4]])
                S.op("dve", lambda e, yb=yb, dc_=dc_, cs=cs: e.scalar_tensor_tensor(out=xT[:, dc_, cs], in0=yb[:], scalar=modc[:, GT2 + dc_:GT2 + dc_ + 1],
                                                                                     in1=xT[:, dc_, cs], op0=ALU.mult, op1=ALU.add),
                     reads=[PB[4 + yi % 4]], writes=["x2_%d_%d" % (dc_, c)])
            if c == NC4 - 1:
                for e_ in grp:
                    p_n = pos_of(e_) + 4
                    nxt = p_n - 1
                    if p_n <= E and (nxt // 2) + 1 < len(groups):
                        load_expert(nxt)
                        if nxt >= 4:
                            load_gates(nxt)

        nsE = len(stepsE)
        for si in range(nsE):
            up_step(si)
            if si == min(1, nsE - 1):
                post_route()
            if si >= 1:
                down_step(si - 1)
        down_step(nsE - 1)
        for c in range(NC4):
            cs = slice(c * 512, (c + 1) * 512)
            for k in range(8):
                S.op("sp", lambda e, k=k, cs=cs: e.dma_start(out=outT_d[k * 128:(k + 1) * 128, cs], in_=xT[:, k, cs]),
                     reads=["x2_%d_%d" % (k, c)], dma_key="out%d" % k)
        return _finish(nc, es, S, A, dbg_out, outT_d)


def _router_mm(e, bank, h2f, wr):
    ins = None
    for t4 in range(4):
        for k in range(8):
            ins = e.matmul(bank[:, t4 * E:(t4 + 1) * E], lhsT=h2f[:, k, t4 * 128:(t4 + 1) * 128], rhs=wr[:, k, :], start=(k == 0), stop=(k == 7))
    return ins


def _finish(nc, es, S, A, dbg_out, outT_d):
    print("[build] sbuf peak bytes/partition:", A.peak, "of", A.TOP)
    S.barrier()
    for name, (t, shape) in dbg_out.items():
        dt = t.dtype
        d = nc.dram_tensor("dbg_" + name, list(shape), dt, kind="ExternalOutput").ap()
        S.op("sp", lambda e, d=d, t=t: e.dma_start(out=d, in_=t[:]), dma_key="out_dbg")
    S.finalize(es)
    with nc.Block() as block:
        S.emit(block)
    return nc


def _col(v, n):
    return np.ascontiguousarray(np.asarray(v, np.float32).reshape(n, 128).T)


def _na_table(rpb):
    rpb = np.asarray(rpb, np.float32)
    p = np.arange(128)
    kr, kcol = p // 64, p % 64
    qc = np.arange(64)
    cstart = np.clip(qc - 8, 0, 48)
    colok = (kcol[:, None] >= cstart[None, :]) & (kcol[:, None] < cstart[None, :] + 16)
    dc = np.clip(kcol[:, None] - qc[None, :], -15, 15) + 15
    tab = np.full((128, 8, 24, 64), NEG, np.float32)
    for n in range(24):
        u = 4 + n if n < 10 else 2 + (n - 10)
        dr = 8 + kr - u
        rowok = ((dr >= -4) & (dr <= 3)) if n < 10 else (np.abs(dr) <= 7)
        ok = rowok[:, None] & colok
        dri = np.clip(dr + 7, 0, 14)
        vals = rpb[:, dri[:, None], dc]
        vals = np.transpose(vals, (1, 0, 2))
        tab[:, :, n, :] = np.where(ok[:, None, :], vals, np.float32(NEG))
    return tab


def _prep_inputs(inputs):
    f = lambda k: np.asarray(inputs[k], np.float32)
    x = f("x")
    B = x.shape[0]
    half = 16
    inv_freq = (10000.0 ** (-np.arange(half, dtype=np.float32) / half)).astype(np.float32)
    ifr = np.zeros((96, 1), np.float32)
    ifr[64:80, 0] = inv_freq / np.float32(2 * np.pi)
    ifr[80:96, 0] = inv_freq / np.float32(2 * np.pi)
    pad96 = lambda v: np.ascontiguousarray(np.asarray(v, np.float32).reshape(96, 1))
    shared = {
        "w_ada": np.ascontiguousarray(f("w_ada")[0]),
        "b_col": _col(f("b_ada")[0], 48),
        "g1_col": _col(f("g_norm1")[0], 8),
        "g2_col": _col(f("g_norm2")[0], 8),
        "w_in": np.ascontiguousarray(f("w_in")[0]),
        "gnaq_col": np.ascontiguousarray(np.tile(f("g_na_q")[0], 2).reshape(128, 1)),
        "gnak_col": np.ascontiguousarray(np.tile(f("g_na_k")[0], 2).reshape(128, 1)),
        "tab": _na_table(f("na_rpb")[0]),
        "gql_col": _col(f("g_q_lat")[0], 2),
        "gkvl_col": _col(f("g_kv_lat")[0], 1),
        "w_uq": np.ascontiguousarray(f("w_uq")[0]),
        "w_ukv": np.ascontiguousarray(f("w_ukv")[0]),
        "gmq_col": pad96(f("g_mla_q")[0]),
        "gmk_col": pad96(f("g_mla_k")[0]),
        "ifr_col": ifr,
        "w_proj_na": np.ascontiguousarray(f("w_proj_na")[0]),
        "w_proj_mla": np.ascontiguousarray(f("w_proj_mla")[0]),
        "w_out": np.ascontiguousarray(f("w_out")[0]),
        "w_router": np.ascontiguousarray(f("w_router")[0]),
        "e_bias": np.ascontiguousarray(f("e_bias")[0].reshape(1, E)),
        "w_exp_gate": np.ascontiguousarray(f("w_exp_gate")[0]),
        "w_exp_up": np.ascontiguousarray(f("w_exp_up")[0]),
        "w_exp_down": np.ascontiguousarray(f("w_exp_down")[0]),
        "w_sh_gate": np.ascontiguousarray(f("w_sh_gate")[0]),
        "w_sh_up": np.ascontiguousarray(f("w_sh_up")[0]),
        "w_sh_down": np.ascontiguousarray(f("w_sh_down")[0]),
    }
    c = f("c")
    pos = np.asarray(inputs["positions"], np.int32)
    maps = []
    for b in range(B):
        m = dict(shared)
        m["xT"] = np.ascontiguousarray(x[b].T)
        m["cT"] = _col(c[b], 8)
        m["pos"] = np.ascontiguousarray(pos[b].reshape(1, S_))
        maps.append(m)
    return maps


_NC_CACHE = {}


def kernel(**inputs):
    maps = _prep_inputs(inputs)
    if "nc" not in _NC_CACHE:
        _NC_CACHE["nc"] = build_nc()
    nc = _NC_CACHE["nc"]
    res = run_bass_kernel_spmd(nc, maps, core_ids=list(range(len(maps))))
    out = np.stack([np.ascontiguousarray(r["outT"].T) for r in res.results], axis=0)
    return out.astype(np.float32)
```

```python
import numpy as np
from contextlib import ExitStack
import concourse.bass as bass
import concourse.mybir as mybir
from concourse.bass_utils import run_bass_kernel_spmd

F32 = mybir.dt.float32
BF16 = mybir.dt.bfloat16
I32 = mybir.dt.int32
ALU = mybir.AluOpType
AF = mybir.ActivationFunctionType
AX = mybir.AxisListType

D = 1024
S_ = 2048
NT = 16
NC4 = 4
E = 64
EPS = 1e-6
NEG = -30000.0
IN_COLS = 4000
C_Q, C_K, C_V, C_QL, C_KVL, C_KR, C_GN, C_GM = 0, 512, 1024, 1536, 1792, 1920, 1952, 2976

ENGS = ("pe", "act", "dve", "pool", "sp")


class _Op:
    __slots__ = ("eng", "fn", "reads", "writes", "dma_key", "waits", "inc", "tok", "idx", "bar")


class Sched:
    def __init__(self, nc):
        self.nc = nc
        self.ops = []
        self._cap = None

    def op(self, eng, fn, reads=(), writes=(), dma_key=None):
        o = _Op()
        o.eng, o.fn, o.reads, o.writes, o.dma_key = eng, fn, tuple(reads), tuple(writes), dma_key
        o.bar = False
        if self._cap is not None:
            self._cap.append(o)
            return o
        o.idx = len(self.ops)
        self.ops.append(o)
        return o

    def begin_unit(self):
        self._cap = []

    def end_unit(self):
        u, self._cap = self._cap, None
        return u

    def emit_units(self, units, shift=None, extra=()):
        n = max(len(u) for u in units)
        shift = shift or (n + 1) // 2
        T = (len(units) - 1) * shift + n
        extra = list(extra)
        every = max(1, T // (len(extra) + 1)) if extra else 0
        for t in range(T):
            for i, u in enumerate(units):
                p = t - i * shift
                if 0 <= p < len(u):
                    o = u[p]
                    o.idx = len(self.ops)
                    self.ops.append(o)
            if extra and t % every == every - 1:
                for o in extra.pop(0):
                    o.idx = len(self.ops)
                    self.ops.append(o)
        for u in extra:
            for o in u:
                o.idx = len(self.ops)
                self.ops.append(o)

    def barrier(self, skip=()):
        o = _Op()
        o.eng, o.fn, o.reads, o.writes, o.dma_key = None, None, (), tuple(skip), None
        o.bar = True
        o.idx = len(self.ops)
        self.ops.append(o)

    def finalize(self, es):
        nc = self.nc
        ops = self.ops
        last_w, readers = {}, {}
        last_comp, last_dma = {}, {}
        pending = {e: set() for e in ENGS}
        deps = [None] * len(ops)
        for o in ops:
            if o.bar:
                allp = set(last_comp.values()) | set(v for k, v in last_dma.items() if k not in o.writes)
                for e in ENGS:
                    pending[e] |= allp
                continue
            d = set()
            for r in o.reads:
                if r in last_w:
                    d.add(last_w[r])
            for w in o.writes:
                if w in last_w:
                    d.add(last_w[w])
                for rr in readers.get(w, ()):
                    d.add(rr)
            d |= pending[o.eng]
            pending[o.eng] = set()
            d.discard(o.idx)
            deps[o.idx] = d
            for r in o.reads:
                readers.setdefault(r, []).append(o.idx)
            for w in o.writes:
                last_w[w] = o.idx
                readers[w] = []
            if o.dma_key is not None:
                last_dma[o.dma_key] = o.idx
            else:
                last_comp[o.eng] = o.idx
        need_inc = [False] * len(ops)
        fdeps = [None] * len(ops)
        for o in ops:
            if o.bar:
                continue
            fd = []
            for pi in deps[o.idx]:
                p = ops[pi]
                if p.dma_key is None and o.dma_key is None and p.eng == o.eng and p.eng == "pe":
                    continue
                fd.append(pi)
                need_inc[pi] = True
            fdeps[o.idx] = fd
        sems, counts = {}, {}

        def get_sem(name):
            if name not in sems:
                sems[name] = es.enter_context(nc.semaphore(name))
                counts[name] = 0

        for o in ops:
            if o.bar:
                continue
            if o.dma_key is not None:
                name = "d_" + o.dma_key
                get_sem(name)
                counts[name] += 16
                o.tok, o.inc = (name, counts[name]), True
            elif need_inc[o.idx]:
                name = "e_" + o.eng
                get_sem(name)
                counts[name] += 1
                o.tok, o.inc = (name, counts[name]), True
            else:
                o.tok, o.inc = None, False
        waited = {e: {} for e in ENGS}
        for o in ops:
            if o.bar:
                continue
            need = {}
            for pi in fdeps[o.idx]:
                name, val = ops[pi].tok
                if need.get(name, 0) < val:
                    need[name] = val
            w = []
            for name, val in need.items():
                if waited[o.eng].get(name, 0) >= val:
                    continue
                waited[o.eng][name] = val
                w.append((name, val))
            o.waits = w
        self.sems, self.counts = sems, counts

    def emit(self, block):
        sems = self.sems
        by = {e: [o for o in self.ops if (not o.bar) and o.eng == e] for e in ENGS}
        final = [(n, c) for n, c in self.counts.items() if n.startswith("d_out")]

        def run(h, lst, fin=None):
            for o in lst:
                for name, val in o.waits:
                    h.wait_ge(sems[name], val)
                ins = o.fn(h)
                if o.inc:
                    ins.then_inc(sems[o.tok[0]], 16 if o.dma_key is not None else 1)
            if fin:
                for name, val in fin:
                    h.wait_ge(sems[name], val)

        @block.tensor
        def _(h):
            run(h, by["pe"])

        @block.scalar
        def _(h):
            run(h, by["act"])

        @block.vector
        def _(h):
            run(h, by["dve"])

        @block.gpsimd
        def _(h):
            run(h, by["pool"])

        @block.sync
        def _(h):
            run(h, by["sp"], final)


class Arena:
    BASE = 16640
    TOP = 228352

    def __init__(self, nc):
        self.nc = nc
        self.off = self.BASE
        self.n = 0
        self.peak = self.off
        self.top = self.TOP

    def alloc_top(self, name, shape, dt):
        esz = 4 if dt in (F32, I32) else 2
        nbytes = (int(np.prod(shape[1:])) * esz + 63) // 64 * 64
        self.top -= nbytes
        assert self.top >= self.off, (name, self.top, self.off)
        self.n += 1
        return self.nc.alloc_sbuf_tensor_at("%s_%d" % (name, self.n), list(shape), dt, offset=self.top)

    def release_top(self):
        self.top = self.TOP

    def alloc(self, name, shape, dt):
        esz = 4 if dt in (F32, I32) else 2
        nbytes = int(np.prod(shape[1:])) * esz
        nbytes = (nbytes + 63) // 64 * 64
        assert self.off + nbytes <= self.top, (name, self.off, nbytes, self.top)
        self.n += 1
        t = self.nc.alloc_sbuf_tensor_at("%s_%d" % (name, self.n), list(shape), dt, offset=self.off)
        self.off += nbytes
        self.peak = max(self.peak, self.off)
        return t

    def mark(self):
        return self.off

    def release(self, m):
        self.off = m


def build_nc(stop_after=None, dbg=()):
    nc = bass.Bass("TRN2", target_bir_lowering=False)
    dram_in = lambda name, shape, dt=F32: nc.dram_tensor(name, list(shape), dt, kind="ExternalInput").ap()
    xT_d = dram_in("xT", [D, S_])
    cT_d = dram_in("cT", [128, 8])
    pos_d = dram_in("pos", [1, S_], I32)
    w_ada_d = dram_in("w_ada", [D, 6 * D])
    bcol_d = dram_in("b_col", [128, 48])
    g1_d = dram_in("g1_col", [128, 8])
    g2_d = dram_in("g2_col", [128, 8])
    w_in_d = dram_in("w_in", [D, IN_COLS])
    gnaq_d = dram_in("gnaq_col", [128, 1])
    gnak_d = dram_in("gnak_col", [128, 1])
    tab_d = dram_in("tab", [128, 8, 24, 64])
    gql_d = dram_in("gql_col", [128, 2])
    gkvl_d = dram_in("gkvl_col", [128, 1])
    w_uq_d = dram_in("w_uq", [256, 768])
    w_ukv_d = dram_in("w_ukv", [128, 1024])
    gmq_d = dram_in("gmq_col", [96, 1])
    gmk_d = dram_in("gmk_col", [96, 1])
    ifr_d = dram_in("ifr_col", [96, 1])
    w_pn_d = dram_in("w_proj_na", [512, D])
    w_pm_d = dram_in("w_proj_mla", [512, D])
    w_out_d = dram_in("w_out", [D, D])
    w_r_d = dram_in("w_router", [D, E])
    ebias_d = dram_in("e_bias", [1, E])
    w_eg_d = dram_in("w_exp_gate", [E, D, 256])
    w_eu_d = dram_in("w_exp_up", [E, D, 256])
    w_ed_d = dram_in("w_exp_down", [E, 256, D])
    w_sg_d = dram_in("w_sh_gate", [D, 256])
    w_su_d = dram_in("w_sh_up", [D, 256])
    w_sd_d = dram_in("w_sh_down", [256, D])
    outT_d = nc.dram_tensor("outT", [D, S_], F32, kind="ExternalOutput").ap()
    dbg_out = {}

    es = ExitStack()
    with es:
        A = Arena(nc)
        S = Sched(nc)
        pall = es.enter_context(nc.psum_tensor("pall", [128, 8, 512], F32))
        pb = [pall[:, i, :] for i in range(8)]
        PB = ["pb%d" % i for i in range(8)]
        kc = lambda d_ap: d_ap.rearrange("(k p) n -> p k n", p=128)

        ident = A.alloc("ident", [128, 128], BF16)
        identf = A.alloc("identf", [128, 128], F32)
        ones = A.alloc("ones", [128, 128], BF16)
        blk = A.alloc("blk", [128, 128], BF16)
        one1 = A.alloc("one1", [1, 16], F32)
        modc = A.alloc("modc", [128, 48], F32)
        A1 = A.alloc("A1", [128, 8], F32)
        A2 = A.alloc("A2", [128, 8], F32)
        cols = A.alloc("cols", [128, 32], F32)
        epsc = A.alloc("epsc", [128, 1], F32)
        GQ, GK, GQL0, GKVL, GMQ, GMK, IFR = 0, 1, 2, 4, 5, 6, 7
        G1C, G2C = 8, 16


        wqkv = A.alloc_top("wqkv", [128, 8, 1536], BF16)
        tab = A.alloc_top("tab", [128, 8, 24, 64], BF16)

        mA = A.mark()
        cT = A.alloc("cT", [128, 8], F32)
        sc = A.alloc("sc", [128, 8], F32)
        bcol = A.alloc("bcol", [128, 48], F32)
        modrow = A.alloc("modrow", [1, 6 * D], F32)
        wada = [A.alloc("wada%d" % i, [128, 8, 512], BF16) for i in range(4)]
        scb = A.alloc("scb", [128, 8], BF16)
        for n in range(4):
            S.op("pool", lambda e, n=n: e.dma_start(out=wada[n][:], in_=kc(w_ada_d[:, n * 512:(n + 1) * 512])),
                 writes=["wada%d" % n], dma_key="wada%d" % n)
        S.op("pool", lambda e: e.memset(ident[:], 0.0), writes=["ident"])
        S.op("pool", lambda e: e.affine_select(out=ident[:], in_=ident[:], pattern=[[-1, 128]], compare_op=ALU.not_equal,
                                               fill=1.0, base=0, channel_multiplier=1), reads=["ident"], writes=["ident"])
        S.op("pool", lambda e: e.memset(identf[:], 0.0), writes=["identf"])
        S.op("pool", lambda e: e.affine_select(out=identf[:], in_=identf[:], pattern=[[-1, 128]], compare_op=ALU.not_equal,
                                               fill=1.0, base=0, channel_multiplier=1), reads=["identf"], writes=["identf"])
        S.op("pool", lambda e: e.memset(ones[:], 1.0), writes=["ones"])
        S.op("pool", lambda e: e.memset(blk[:], 0.0), writes=["blk"])
        S.op("pool", lambda e: e.memset(blk[0:64, 0:64], 1.0), reads=["blk"], writes=["blk"])
        S.op("pool", lambda e: e.memset(blk[64:128, 64:128], 1.0), reads=["blk"], writes=["blk"])
        S.op("pool", lambda e: e.memset(one1[:], 1.0), writes=["one1"])
        S.op("pool", lambda e: e.memset(epsc[:], EPS), writes=["epsc"])
        S.op("pool", lambda e: e.memset(cols[:], 0.0), writes=["cols"])
        small = [(gnaq_d, GQ, 128, 1), (gnak_d, GK, 128, 1), (gql_d, GQL0, 128, 2), (gkvl_d, GKVL, 128, 1),
                 (gmq_d, GMQ, 96, 1), (gmk_d, GMK, 96, 1), (ifr_d, IFR, 96, 1), (g1_d, G1C, 128, 8), (g2_d, G2C, 128, 8)]
        for i, (src, c0, npart, w) in enumerate(small):
            S.op("sp", lambda e, src=src, c0=c0, npart=npart, w=w: e.dma_start(out=cols[0:npart, c0:c0 + w], in_=src),
                 reads=["cols"], writes=["cols%d" % i], dma_key="cols")
        COLS = ["cols%d" % i for i in range(len(small))]
        S.op("dve", lambda e: e.tensor_scalar(out=cols[:, GQ:GQ + 1], in0=cols[:, GQ:GQ + 1], scalar1=64 ** -0.5, scalar2=None, op0=ALU.mult),
             reads=COLS, writes=["colsx"])
        S.op("dve", lambda e: e.tensor_scalar(out=cols[0:96, GMQ:GMQ + 1], in0=cols[0:96, GMQ:GMQ + 1], scalar1=96 ** -0.5, scalar2=None, op0=ALU.mult),
             reads=COLS + ["colsx"], writes=["colsy"])
        COLS = COLS + ["colsx", "colsy"]
        S.op("sp", lambda e: e.dma_start(out=cT[:], in_=cT_d), writes=["cT"], dma_key="cT")
        S.op("sp", lambda e: e.dma_start(out=bcol[:], in_=bcol_d), writes=["bcol"], dma_key="bcol")
        S.op("act", lambda e: e.activation(out=scb[:], in_=cT[:], func=AF.Silu), reads=["cT"], writes=["sc"])
        def mod_transpose(n):
            def fn(e, n=n):
                ins = None
                for j in range(4 * n, 4 * n + 4):
                    ins = e.matmul(pb[2][:, j:j + 1], lhsT=modrow[0:1, j * 128:(j + 1) * 128], rhs=one1[0:1, 0:1], start=True, stop=True)
                return ins
            S.op("pe", fn, reads=["modrow%d" % n, "one1"], writes=[PB[2]])

        for n in range(12):
            wb = wada[n % 4]
            if n >= 4:
                S.op("pool", lambda e, wb=wb, n=n: e.dma_start(out=wb[:], in_=kc(w_ada_d[:, n * 512:(n + 1) * 512])),
                     writes=["wada%d" % (n % 4)], dma_key="wada%d" % (n % 4))

            def fn(e, wb=wb, n=n):
                ins = None
                for k in range(8):
                    ins = e.matmul(pb[n % 2][0:1, :], lhsT=scb[:, k:k + 1], rhs=wb[:, k, :], start=(k == 0), stop=(k == 7))
                return ins
            S.op("pe", fn, reads=["sc", "wada%d" % (n % 4)], writes=[PB[n % 2]])
            S.op("act", lambda e, n=n: e.copy(out=modrow[0:1, n * 512:(n + 1) * 512], in_=pb[n % 2][0:1, :]),
                 reads=[PB[n % 2]], writes=["modrow%d" % n])
            if n >= 1:
                mod_transpose(n - 1)
        mod_transpose(11)
        S.op("dve", lambda e: e.tensor_tensor(out=modc[:], in0=pb[2][:, 0:48], in1=bcol[:], op=ALU.add), reads=[PB[2], "bcol"], writes=["modc"])
        S.op("dve", lambda e: e.scalar_tensor_tensor(out=A1[:], in0=modc[:, 8:16], scalar=1.0, in1=cols[:, G1C:G1C + 8], op0=ALU.add, op1=ALU.mult),
             reads=["modc"] + COLS, writes=["A1"])
        S.op("dve", lambda e: e.scalar_tensor_tensor(out=A2[:], in0=modc[:, 32:40], scalar=1.0, in1=cols[:, G2C:G2C + 8], op0=ALU.add, op1=ALU.mult),
             reads=["modc"] + COLS, writes=["A2"])
        SH1, GT1, SH2, GT2 = 0, 16, 24, 40
        if "modc" in dbg:
            dbg_out["modc"] = (modc, [128, 48])
        S.barrier()
        A.release(mA)
        if stop_after == "A":
            return _finish(nc, es, S, A, dbg_out, outT_d)

        r1 = A.mark()
        xT = A.alloc("xT", [128, 8, S_], F32)
        hT = nc.alloc_sbuf_tensor_at("hT_al", [128, 8, S_], BF16, offset=r1)
        yT_na = nc.alloc_sbuf_tensor_at("yTna_al", [128, 4, S_], BF16, offset=r1 + 32768)
        yT_mla = nc.alloc_sbuf_tensor_at("yTmla_al", [128, 4, S_], BF16, offset=r1 + 49152)

        def rinv_from(ss_ap, rs_ap, rinv_ap, n_feat, reads, wname, np_=128):
            S.op("act", lambda e: e.activation(out=rs_ap, in_=ss_ap, func=AF.Ln, bias=epsc[0:np_, :], scale=1.0 / n_feat),
                 reads=reads + ["epsc"], writes=[wname + "_rs"])
            S.op("act", lambda e: e.activation(out=rinv_ap, in_=rs_ap, func=AF.Exp, scale=-0.5), reads=[wname + "_rs"], writes=[wname])

        mB = A.mark()
        xch = [A.alloc("xch%d" % i, [128, 8, 512], F32) for i in range(2)]
        sqb = [A.alloc("sqb%d" % i, [128, 512], BF16) for i in range(2)]
        rs = [A.alloc("rs%d" % i, [128, 512], F32) for i in range(2)]
        rinv = [A.alloc("rinv%d" % i, [128, 512], F32) for i in range(2)]
        tmp = [A.alloc("tmp%d" % i, [128, 512], F32) for i in range(2)]
        b1_units = []
        for c in range(NC4):
            S.begin_unit()
            xb = xch[c % 2]
            cs = slice(c * 512, (c + 1) * 512)
            for k in range(8):
                S.op("sp", lambda e, xb=xb, k=k, cs=cs: e.dma_start(out=xb[:, k, :], in_=xT_d[k * 128:(k + 1) * 128, cs]),
                     writes=["xch%d_%d" % (c % 2, k)], dma_key="xch%d_%d" % (c % 2, k))
            for k in range(8):
                S.op("act", lambda e, xb=xb, k=k: e.activation(out=sqb[k % 2][:], in_=xb[:, k, :], func=AF.Square),
                     reads=["xch%d_%d" % (c % 2, k)], writes=["sqb%d" % (k % 2)])
                S.op("pe", lambda e, k=k, c=c: e.matmul(pb[c % 2][:], lhsT=ones[:], rhs=sqb[k % 2][:], start=(k == 0), stop=(k == 7)),
                     reads=["sqb%d" % (k % 2), "ones"], writes=[PB[c % 2]])
            rinv_from(pb[c % 2][:], rs[c % 2][:], rinv[c % 2][:], D, [PB[c % 2]], "rinv%d" % (c % 2))
            for k in range(8):
                S.op("dve", lambda e, xb=xb, k=k, c=c: e.scalar_tensor_tensor(out=tmp[k % 2][:], in0=xb[:, k, :], scalar=A1[:, k:k + 1],
                                                                              in1=rinv[c % 2][:], op0=ALU.mult, op1=ALU.mult),
                     reads=["xch%d_%d" % (c % 2, k), "A1", "rinv%d" % (c % 2)], writes=["tmp%d" % (k % 2)])
                S.op("act", lambda e, k=k, cs=cs: e.activation(out=hT[:, k, cs], in_=tmp[k % 2][:], func=AF.Identity,
                                                                bias=modc[:, SH1 + k:SH1 + k + 1], scale=1.0),
                     reads=["tmp%d" % (k % 2), "modc"], writes=["hT%d_%d" % (c, k)])
            b1_units.append(S.end_unit())
        S.emit_units(b1_units)
        XLAST = ["xch%d_%d" % (c2_, k) for c2_ in range(2) for k in range(8)]
        for g in range(3):
            S.op("pool", lambda e, g=g: e.dma_start(out=wqkv[:, :, g * 512:(g + 1) * 512], in_=kc(w_in_d[:, g * 512:(g + 1) * 512])),
                 reads=XLAST, writes=["wqkv%d" % g], dma_key="wqkv%d" % g)
        S.op("pool", lambda e: e.dma_start(out=tab[:], in_=tab_d), reads=XLAST, writes=["tab0"], dma_key="tab")
        HT = lambda c: ["hT%d_%d" % (c, k) for k in range(8)]
        if "hT" in dbg:
            dbg_out["hT"] = (hT, [128, 8, S_])
        S.barrier(skip=("wqkv0", "wqkv1", "wqkv2", "tab"))
        A.release(mB)
        if stop_after == "B1":
            return _finish(nc, es, S, A, dbg_out, outT_d)

        mNA = A.mark()
        qT_na = A.alloc("qz_na", [128, 8, S_], BF16)
        kT_na = A.alloc("kT_na", [128, 4, S_], BF16)
        v_na = A.alloc("v_na", [128, NT, 8, 65], BF16)
        S.op("pool", lambda e: e.memset(qT_na[:], 0.0), writes=["qz_zero"])
        sqn = [A.alloc("sqn%d" % i, [128, 512], BF16) for i in range(3)]
        rsn = [A.alloc("rsn%d" % i, [128, 512], F32) for i in range(3)]
        rinvn = [A.alloc("rinvn%d" % i, [128, 512], F32) for i in range(3)]
        S.op("pool", lambda e: e.memset(v_na[:, :, :, 64:65], 1.0), writes=["v_na_ones"])
        ui = 0
        na_units = []
        for which, dst, gcol in ((0, qT_na, GQ), (1, kT_na, GK)):
            for p in range(4):
                for c in range(NC4):
                    S.begin_unit()
                    cs = slice(c * 512, (c + 1) * 512)
                    bb = (0, 2, 6)[ui % 3]
                    b0, b1 = pb[bb], pb[bb + 1]
                    n0, n1 = PB[bb], PB[bb + 1]
                    col0 = which * 512 + p * 128

                    def fn(e, b0=b0, col0=col0, cs=cs):
                        ins = None
                        for k in range(8):
                            ins = e.matmul(b0[:], lhsT=wqkv[:, k, col0:col0 + 128], rhs=hT[:, k, cs], start=(k == 0), stop=(k == 7))
                        return ins
                    S.op("pe", fn, reads=["wqkv%d" % which] + HT(c), writes=[n0])
                    u2 = ui % 3
                    S.op("act", lambda e, b0=b0, u2=u2: e.activation(out=sqn[u2][:], in_=b0[:], func=AF.Square), reads=[n0], writes=["sqn%d" % u2])
                    S.op("pe", lambda e, b1=b1, u2=u2: e.matmul(b1[:], lhsT=blk[:], rhs=sqn[u2][:], start=True, stop=True),
                         reads=["sqn%d" % u2, "blk"], writes=[n1])
                    rinv_from(b1[:], rsn[u2][:], rinvn[u2][:], 64, [n1], "rinvn%d" % u2)
                    if which == 1:
                        S.op("dve", lambda e, b0=b0, u2=u2, dst=dst, p=p, cs=cs, gcol=gcol: e.scalar_tensor_tensor(
                            out=dst[:, p, cs], in0=b0[:], scalar=cols[:, gcol:gcol + 1], in1=rinvn[u2][:], op0=ALU.mult, op1=ALU.mult),
                            reads=[n0, "rinvn%d" % u2] + COLS, writes=["qk_%d_%d_%d" % (which, p, c)])
                    else:
                        for par in range(2):
                            rr = slice(par * 64, (par + 1) * 64)
                            S.op("dve", lambda e, b0=b0, u2=u2, dst=dst, p=p, cs=cs, gcol=gcol, rr=rr, par=par: e.scalar_tensor_tensor(
                                out=dst[rr, 2 * p + par, cs], in0=b0[rr, :], scalar=cols[rr, gcol:gcol + 1], in1=rinvn[u2][rr, :], op0=ALU.mult, op1=ALU.mult),
                                reads=[n0, "rinvn%d" % u2, "qz_zero"] + COLS, writes=["qk_%d_%d_%d_%d" % (which, p, c, par)])
                    ui += 1
                    na_units.append(S.end_unit())
        v_units = []
        for t in range(NT):
            S.begin_unit()
            b = pb[4 + t % 2]

            def fn(e, b=b, t=t):
                ins = None
                for k in range(8):
                    ins = e.matmul(b[:], lhsT=hT[:, k, t * 128:(t + 1) * 128], rhs=wqkv[:, k, 1024:1536], start=(k == 0), stop=(k == 7))
                return ins
            S.op("pe", fn, reads=["wqkv2"] + HT(t // 4), writes=[PB[4 + t % 2]])
            S.op("act", lambda e, b=b, t=t: e.copy(out=v_na[:, t, :, 0:64], in_=b[:].rearrange("p (h d) -> p h d", d=64)),
                 reads=[PB[4 + t % 2]], writes=["v_na%d" % t])
            v_units.append(S.end_unit())
        mixed = []
        for i, u in enumerate(na_units):
            mixed.append(u)
        S.emit_units(na_units, shift=(max(len(u) for u in na_units) + 2) // 3, extra=v_units)
        if "qT_na" in dbg:
            dbg_out["qT_na"] = (qT_na, [128, 8, S_])
            dbg_out["kT_na"] = (kT_na, [128, 4, S_])
            dbg_out["v_na"] = (v_na, [128, NT, 8, 65])
        S.barrier()
        if stop_after == "NAprep":
            return _finish(nc, es, S, A, dbg_out, outT_d)
        pT = [A.alloc("pT%d" % i, [128, 640], BF16) for i in range(2)]
        rden = [A.alloc("rden%d" % i, [128, 8], F32) for i in range(2)]
        ytok = [A.alloc("ytok%d" % i, [128, 8, 64], BF16) for i in range(2)]
        units = []
        for t in range(NT):
            if 2 <= t <= 13:
                js = [t + 2 - s for s in range(5)]
                ib = 0 + 0
                tbase, idx0 = 0, 0
            else:
                jmax = 3 if t < 2 else 15
                js = [jmax - s for s in range(4)]
                u0 = 8 - 2 * (jmax - t)
                tbase, idx0 = 10, u0 - 2
            for h in range(8):
                units.append((t, h, js, tbase + idx0))
        o_ps = [pb[4], pb[5]]
        O_PS = [PB[4], PB[5]]

        def emit_qk(i):
            t, h, js, ti0 = units[i]
            p, par = h // 2, h % 2
            r0 = par * 64
            sA, sB = pb[(i % 2) * 2], pb[(i % 2) * 2 + 1]

            def fn(e):
                ins = None
                for s, j in enumerate(js):
                    dst = sA[:, s * 128:(s + 1) * 128] if s < 4 else sB[:, 0:128]
                    e.matmul(dst, lhsT=kT_na[:, p, j * 128:(j + 1) * 128], rhs=qT_na[:, h, t * 128:(t + 1) * 128],
                             start=True, stop=False)
                    ins = e.matmul(dst, lhsT=ident[:], rhs=tab[:, h, ti0 + 2 * s:ti0 + 2 * s + 2, :], start=False, stop=True)
                return ins
            S.op("pe", fn, reads=[], writes=[PB[(i % 2) * 2], PB[(i % 2) * 2 + 1]])
            nj = len(js)
            b0i = (i % 2) * 2
            if nj == 5:
                flat = pall[:, b0i:b0i + 2, :].rearrange("p b n -> p (b n)")
                S.op("act", lambda e: e.activation(out=pT[i % 2][:, 0:640], in_=flat[:, 0:640], func=AF.Exp),
                     reads=[PB[b0i], PB[b0i + 1]], writes=["pTa%d" % (i % 2), "pTb%d" % (i % 2)])
            else:
                S.op("act", lambda e: e.activation(out=pT[i % 2][:, 0:512], in_=sA[:], func=AF.Exp),
                     reads=[PB[b0i]], writes=["pTa%d" % (i % 2)])

        def emit_pv(i):
            t, h, js, ti0 = units[i]
            ob = o_ps[h // 4]
            hh = h % 4

            def fn(e):
                ins = None
                for s, j in enumerate(js):
                    ins = e.matmul(ob[:, hh * 65:(hh + 1) * 65], lhsT=pT[i % 2][:, s * 128:(s + 1) * 128], rhs=v_na[:, j, h, :],
                                   start=(s == 0), stop=(s == len(js) - 1))
                return ins
            S.op("pe", fn, reads=["pTa%d" % (i % 2), "pTb%d" % (i % 2)], writes=[O_PS[h // 4]])
            if h == 7:
                t2 = t % 2
                for half in range(2):
                    ov = o_ps[half][:, 0:260].rearrange("p (h d) -> p h d", d=65)
                    S.op("dve", lambda e, ov=ov, half=half, t2=t2: e.reciprocal(out=rden[t2][:, half * 4:(half + 1) * 4], in_=ov[:, :, 64]),
                         reads=[O_PS[half]], writes=["rden%d_%d" % (t2, half)])
                    S.op("dve", lambda e, ov=ov, half=half, t2=t2: e.tensor_tensor(
                        out=ytok[t2][:, half * 4:(half + 1) * 4, :], in0=ov[:, :, 0:64],
                        in1=rden[t2][:, half * 4:(half + 1) * 4].unsqueeze(2).to_broadcast([128, 4, 64]), op=ALU.mult),
                        reads=[O_PS[half], "rden%d_%d" % (t2, half)], writes=["ytok%d_%d" % (t2, half)])
                trb = pb[6][:].bitcast(BF16)

                def fnt(e, t2=t2):
                    ins = None
                    yv = ytok[t2][:].rearrange("p h d -> p (h d)")
                    for bq in range(4):
                        ins = e.transpose(out=trb[:, bq * 128:(bq + 1) * 128], in_=yv[:, bq * 128:(bq + 1) * 128], identity=ident[:])
                    return ins
                S.op("pe", fnt, reads=["ytok%d_0" % t2, "ytok%d_1" % t2], writes=[PB[6]])
                S.op("act", lambda e, t=t: e.copy(out=yT_na[:, :, t * 128:(t + 1) * 128], in_=trb[:, 0:512].rearrange("p (b n) -> p b n", n=128)),
                     reads=[PB[6]], writes=["yT_na%d" % t])

        nu = len(units)
        emit_qk(0)
        for i in range(nu):
            if i + 1 < nu:
                emit_qk(i + 1)
            emit_pv(i)
        if "yT_na" in dbg:
            dbg_out["yT_na"] = (yT_na, [128, 4, S_])
        S.barrier()
        A.release(mNA)
        A.release_top()
        if stop_after == "NA":
            return _finish(nc, es, S, A, dbg_out, outT_d)

        mM = A.mark()
        qmT = A.alloc("qmT", [96, 8, S_], BF16)
        kmT = A.alloc("kmT", [96, 8, S_], BF16)
        kvlatT = A.alloc("kvlatT", [128, S_], BF16)
        wukv = A.alloc("wukv", [128, 1024], BF16)
        mMp = A.mark()
        qlatT = A.alloc("qlatT", [128, 2, S_], BF16)
        Ct = A.alloc("Ct", [96, S_], F32)
        Sn = A.alloc("Sn", [96, S_], F32)
        krope = A.alloc("krope", [96, S_], F32)
        mM1 = A.mark()
        wg3 = A.alloc("wg3", [128, 8, 416], BF16)
        wkr = A.alloc("wkr", [128, 8, 96], BF16)
        wkrr = A.alloc("wkrr", [128, 8, 96], BF16)
        posi = A.alloc("posi", [96, 512], I32)
        rndi = A.alloc("rndi", [96, 512], I32)
        ang = A.alloc("ang", [96, 512], F32)
        angf = A.alloc("angf", [96, 512], F32)
        sq96 = [A.alloc("sq96_%d" % i, [128, 512], BF16) for i in range(2)]
        sq96b = [A.alloc("sq96b_%d" % i, [128, 512], BF16) for i in range(2)]
        rs96 = [A.alloc("rs96_%d" % i, [128, 512], F32) for i in range(2)]
        rinv96 = [A.alloc("rinv96_%d" % i, [128, 512], F32) for i in range(2)]
        qraw = [A.alloc("qraw%d" % i, [96, 512], F32) for i in range(2)]
        tmp2 = [A.alloc("tmp2_%d" % i, [96, 512], F32) for i in range(2)]
        S.op("pool", lambda e: e.dma_start(out=wg3[:], in_=kc(w_in_d[:, C_QL:C_GN])), writes=["wg3"], dma_key="wg3")
        S.op("pool", lambda e: e.dma_start(out=wukv[:], in_=w_ukv_d), writes=["wukv"], dma_key="wukv")
        TWO_PI = 6.2831
        for c in range(NC4):
            cs = slice(c * 512, (c + 1) * 512)
            S.op("sp", lambda e, cs=cs: e.dma_start(out=posi[:], in_=pos_d[:, cs].to_broadcast([96, 512])), writes=["posi"], dma_key="posi")
            S.op("dve", lambda e: e.tensor_copy(out=angf[:], in_=posi[:]), reads=["posi"], writes=["angf"])
            S.op("dve", lambda e: e.tensor_scalar(out=ang[:], in0=angf[:], scalar1=cols[0:96, IFR:IFR + 1], scalar2=None, op0=ALU.mult),
                 reads=["angf"] + COLS, writes=["ang"])
            for which, dst, shift in ((0, Sn, 0.0), (1, Ct, 0.25)):
                if shift != 0.0:
                    S.op("dve", lambda e, shift=shift: e.tensor_scalar(out=ang[:], in0=ang[:], scalar1=shift, scalar2=None, op0=ALU.add),
                         reads=["ang"], writes=["ang"])
                S.op("dve", lambda e: e.tensor_copy(out=rndi[:], in_=ang[:]), reads=["ang"], writes=["rndi"])
                S.op("dve", lambda e: e.tensor_copy(out=angf[:], in_=rndi[:]), reads=["rndi"], writes=["angf"])
                S.op("dve", lambda e: e.tensor_tensor(out=angf[:], in0=ang[:], in1=angf[:], op=ALU.subtract), reads=["ang", "angf"], writes=["angf"])
                S.op("act", lambda e, dst=dst, cs=cs: e.activation(out=dst[:, cs], in_=angf[:], func=AF.Sin, scale=TWO_PI), reads=["angf"],
                     writes=["Sn%d" % c if which == 0 else "Ct%d" % c])
        S.op("pool", lambda e: e.memset(wkr[:], 0.0), writes=["wkr"])
        S.op("pool", lambda e: e.memset(wkrr[:], 0.0), writes=["wkrr"])
        S.op("pool", lambda e: e.tensor_copy(out=wkr[:, :, 64:96], in_=wg3[:, :, 384:416]), reads=["wg3", "wkr"], writes=["wkr"])
        S.op("pool", lambda e: e.tensor_scalar(out=wkrr[:, :, 64:80], in0=wg3[:, :, 400:416], scalar1=-1.0, scalar2=None, op0=ALU.mult),
             reads=["wg3", "wkrr"], writes=["wkrr"])
        S.op("pool", lambda e: e.tensor_copy(out=wkrr[:, :, 80:96], in_=wg3[:, :, 384:400]), reads=["wg3", "wkrr"], writes=["wkrr"])
        lat_units = []
        for c in range(NC4):
            S.begin_unit()
            cs = slice(c * 512, (c + 1) * 512)
            c2 = c % 2
            B0, B1, B2, B3 = [pb[4 * c2 + i] for i in range(4)]
            N0, N1, N2, N3 = [PB[4 * c2 + i] for i in range(4)]

            def proj(bank, col0, ncol, w=None, cs=cs):
                def fn(e):
                    ins = None
                    for k in range(8):
                        lhs = wg3[:, k, col0:col0 + ncol] if w is None else w[:, k, :]
                        ins = e.matmul(bank[0:ncol, :], lhsT=lhs, rhs=hT[:, k, cs], start=(k == 0), stop=(k == 7))
                    return ins
                return fn
            S.op("pe", proj(B0, 0, 128), reads=["wg3"] + HT(c), writes=[N0])
            S.op("pe", proj(B1, 128, 128), reads=["wg3"] + HT(c), writes=[N1])
            S.op("act", lambda e, c2=c2, B0=B0: e.activation(out=sq96[c2][:], in_=B0[:], func=AF.Square), reads=[N0], writes=["sq96_%d" % c2])
            S.op("act", lambda e, c2=c2, B1=B1: e.activation(out=sq96b[c2][:], in_=B1[:], func=AF.Square), reads=[N1], writes=["sq96b_%d" % c2])

            def fn(e, c2=c2, B2=B2):
                e.matmul(B2[:], lhsT=ones[:], rhs=sq96[c2][:], start=True, stop=False)
                return e.matmul(B2[:], lhsT=ones[:], rhs=sq96b[c2][:], start=False, stop=True)
            S.op("pe", fn, reads=["sq96_%d" % c2, "sq96b_%d" % c2, "ones"], writes=[N2])
            rinv_from(B2[:], rs96[c2][:], rinv96[c2][:], 256, [N2], "rinv96_%d" % c2)
            for j, (Bj, Nj) in enumerate(((B0, N0), (B1, N1))):
                S.op("dve", lambda e, j=j, c2=c2, cs=cs, Bj=Bj: e.scalar_tensor_tensor(out=qlatT[:, j, cs], in0=Bj[:], scalar=cols[:, GQL0 + j:GQL0 + j + 1],
                                                                                         in1=rinv96[c2][:], op0=ALU.mult, op1=ALU.mult),
                     reads=[Nj, "rinv96_%d" % c2] + COLS, writes=["qlatT%d_%d" % (c, j)])
            S.op("pe", proj(B3, 256, 128), reads=["wg3"] + HT(c), writes=[N3])
            S.op("act", lambda e, c2=c2, B3=B3: e.activation(out=sq96[c2][:], in_=B3[:], func=AF.Square), reads=[N3], writes=["sq96_%d" % c2])
            S.op("pe", lambda e, c2=c2, B2=B2: e.matmul(B2[:], lhsT=ones[:], rhs=sq96[c2][:], start=True, stop=True),
                 reads=["sq96_%d" % c2, "ones"], writes=[N2])
            rinv_from(B2[:], rs96[c2][:], rinv96[c2][:], 128, [N2], "rinv96_%d" % c2)
            S.op("dve", lambda e, c2=c2, cs=cs, B3=B3: e.scalar_tensor_tensor(out=kvlatT[:, cs], in0=B3[:], scalar=cols[:, GKVL:GKVL + 1],
                                                                               in1=rinv96[c2][:], op0=ALU.mult, op1=ALU.mult),
                 reads=[N3, "rinv96_%d" % c2] + COLS, writes=["kvlatT%d" % c])
            S.op("pe", proj(B0, 0, 96, wkr), reads=["wkr"] + HT(c), writes=[N0])
            S.op("pe", proj(B1, 0, 96, wkrr), reads=["wkrr"] + HT(c), writes=[N1])
            S.op("dve", lambda e, c2=c2, cs=cs, B0=B0: e.tensor_tensor(out=qraw[c2][:], in0=B0[0:96, :], in1=Ct[:, cs], op=ALU.mult),
                 reads=[N0, "Ct%d" % c], writes=["qraw%d" % c2])
            S.op("dve", lambda e, c2=c2, cs=cs, B1=B1: e.tensor_tensor(out=tmp2[c2][:], in0=B1[0:96, :], in1=Sn[:, cs], op=ALU.mult),
                 reads=[N1, "Sn%d" % c], writes=["tmp2_%d" % c2])
            S.op("dve", lambda e, c2=c2, cs=cs: e.tensor_tensor(out=krope[:, cs], in0=qraw[c2][:], in1=tmp2[c2][:], op=ALU.add),
                 reads=["qraw%d" % c2, "tmp2_%d" % c2], writes=["krope%d" % c])
            lat_units.append(S.end_unit())
        S.emit_units(lat_units)
        S.barrier()
        A.release(mM1)
        wuq = A.alloc("wuq", [128, 2, 768], BF16)
        wuqr = A.alloc("wuqr", [128, 2, 768], BF16)
        sqU = [A.alloc("sqU_%d" % i, [128, 2, 512], BF16) for i in range(2)]
        rsU = [A.alloc("rsU_%d" % i, [128, 2, 512], F32) for i in range(2)]
        qrawU = [A.alloc("qrawU%d" % i, [96, 2, 512], F32) for i in range(2)]
        tmpU = [A.alloc("tmpU_%d" % i, [96, 2, 512], F32) for i in range(1)] * 2
        sqk = [A.alloc("sqk_%d" % i, [96, 2, 512], BF16) for i in range(2)]
        sqr = A.alloc("sqr", [96, S_], BF16)
        S.op("pool", lambda e: e.memset(sqk[0][:], 0.0), writes=["sqk0"])
        S.op("pool", lambda e: e.memset(sqk[1][:], 0.0), writes=["sqk1"])
        S.op("pool", lambda e: e.memset(sqr[:], 0.0), writes=["sqr"])
        S.op("act", lambda e: e.activation(out=sqr[64:96, :], in_=krope[64:96, :], func=AF.Square), reads=["sqr"], writes=["sqr"])
        S.op("pool", lambda e: e.dma_start(out=wuq[:], in_=kc(w_uq_d)), writes=["wuq"], dma_key="wuq")
        wq4 = wuq[:].rearrange("p j (h d) -> p j h d", d=96)
        wr4 = wuqr[:].rearrange("p j (h d) -> p j h d", d=96)
        S.op("pool", lambda e: e.memset(wuqr[:], 0.0), writes=["wuqr"])
        for j in range(2):
            S.op("pool", lambda e, j=j: e.tensor_scalar(out=wr4[:, j, :, 64:80], in0=wq4[:, j, :, 80:96], scalar1=-1.0, scalar2=None, op0=ALU.mult),
                 reads=["wuq", "wuqr"], writes=["wuqr"])
            S.op("pool", lambda e, j=j: e.tensor_copy(out=wr4[:, j, :, 80:96], in_=wq4[:, j, :, 64:80]), reads=["wuq", "wuqr"], writes=["wuqr"])
        ui = 0
        mla_units = []
        for c in range(NC4):
            cs = slice(c * 512, (c + 1) * 512)
            for hp in range(4):
                S.begin_unit()
                h0 = 2 * hp
                u2 = ui % 2
                base = u2 * 4
                nAB = [PB[base + i] for i in range(4)]
                bcC = Ct[:, cs].unsqueeze(1).to_broadcast([96, 2, 512])
                bcS = Sn[:, cs].unsqueeze(1).to_broadcast([96, 2, 512])

                def fnq(e, w, off, base=base, h0=h0, cs=cs):
                    ins = None
                    for hh in range(2):
                        h = h0 + hh
                        e.matmul(pall[0:96, base + off + hh, :], lhsT=w[:, 0, h * 96:(h + 1) * 96], rhs=qlatT[:, 0, cs], start=True, stop=False)
                        ins = e.matmul(pall[0:96, base + off + hh, :], lhsT=w[:, 1, h * 96:(h + 1) * 96], rhs=qlatT[:, 1, cs], start=False, stop=True)
                    return ins
                S.op("pe", lambda e, f=fnq: f(e, wuq, 0), reads=["wuq"], writes=nAB[0:2])
                S.op("pe", lambda e, f=fnq: f(e, wuqr, 2), reads=["wuqr"], writes=nAB[2:4])
                S.op("dve", lambda e, base=base, u2=u2, bcC=bcC: e.tensor_tensor(out=qrawU[u2][:], in0=pall[0:96, base:base + 2, :], in1=bcC, op=ALU.mult),
                     reads=nAB[0:2], writes=["qrawU%d" % u2])
                S.op("dve", lambda e, base=base, u2=u2, bcS=bcS: e.tensor_tensor(out=tmpU[u2][:], in0=pall[0:96, base + 2:base + 4, :], in1=bcS, op=ALU.mult),
                     reads=nAB[2:4], writes=["tmpU0"])
                S.op("dve", lambda e, u2=u2: e.tensor_tensor(out=qrawU[u2][:], in0=qrawU[u2][:], in1=tmpU[u2][:], op=ALU.add),
                     reads=["qrawU%d" % u2, "tmpU0"], writes=["qrawU%d" % u2])
                S.op("act", lambda e, u2=u2: e.activation(out=sqU[u2][0:96], in_=qrawU[u2][:], func=AF.Square),
                     reads=["qrawU%d" % u2], writes=["sqU%d" % u2])

                def fss(e, base=base, u2=u2):
                    ins = None
                    for hh in range(2):
                        ins = e.matmul(pall[0:96, base + hh, :], lhsT=ones[0:96, 0:96], rhs=sqU[u2][0:96, hh, :], start=True, stop=True)
                    return ins
                S.op("pe", fss, reads=["sqU%d" % u2], writes=nAB[0:2])
                S.op("act", lambda e, base=base, u2=u2: e.activation(out=rsU[u2][0:96], in_=pall[0:96, base:base + 2, :], func=AF.Ln,
                                                                      bias=epsc[0:96, :], scale=1.0 / 96), reads=nAB[0:2], writes=["rsU%d" % u2])
                S.op("act", lambda e, u2=u2: e.activation(out=rsU[u2][0:96], in_=rsU[u2][0:96], func=AF.Exp, scale=-0.5), reads=["rsU%d" % u2], writes=["rsU%d" % u2])
                S.op("dve", lambda e, u2=u2, h0=h0, cs=cs: e.scalar_tensor_tensor(out=qmT[:, h0:h0 + 2, cs], in0=qrawU[u2][:], scalar=cols[0:96, GMQ:GMQ + 1],
                                                                                   in1=rsU[u2][0:96], op0=ALU.mult, op1=ALU.mult),
                     reads=["qrawU%d" % u2, "rsU%d" % u2], writes=["qmT%d_%d" % (hp, c)])
                def fk(e, base=base, h0=h0, cs=cs):
                    ins = None
                    for hh in range(2):
                        h = h0 + hh
                        ins = e.matmul(pall[0:64, base + 2 + hh, :], lhsT=wukv[:, h * 128:h * 128 + 64], rhs=kvlatT[:, cs], start=True, stop=True)
                    return ins
                S.op("pe", fk, reads=["wukv"], writes=nAB[2:4])
                S.op("act", lambda e, base=base, u2=u2: e.activation(out=sqk[u2][0:64], in_=pall[0:64, base + 2:base + 4, :], func=AF.Square),
                     reads=nAB[2:4] + ["sqk%d" % u2], writes=["sqk%d" % u2])

                def fssk(e, base=base, u2=u2, cs=cs):
                    ins = None
                    for hh in range(2):
                        e.matmul(pall[0:96, base + hh, :], lhsT=ones[0:96, 0:96], rhs=sqk[u2][0:96, hh, :], start=True, stop=False)
                        ins = e.matmul(pall[0:96, base + hh, :], lhsT=ones[0:96, 0:96], rhs=sqr[0:96, cs], start=False, stop=True)
                    return ins
                S.op("pe", fssk, reads=["sqk%d" % u2, "sqr"], writes=nAB[0:2])
                S.op("act", lambda e, base=base, u2=u2: e.activation(out=rsU[u2][0:96], in_=pall[0:96, base:base + 2, :], func=AF.Ln,
                                                                      bias=epsc[0:96, :], scale=1.0 / 96), reads=nAB[0:2], writes=["rsU%d" % u2])
                S.op("act", lambda e, u2=u2: e.activation(out=rsU[u2][0:96], in_=rsU[u2][0:96], func=AF.Exp, scale=-0.5), reads=["rsU%d" % u2], writes=["rsU%d" % u2])
                S.op("dve", lambda e, base=base, u2=u2, h0=h0, cs=cs: e.scalar_tensor_tensor(out=kmT[0:64, h0:h0 + 2, cs], in0=pall[0:64, base + 2:base + 4, :],
                                                                                              scalar=cols[0:64, GMK:GMK + 1], in1=rsU[u2][0:64], op0=ALU.mult, op1=ALU.mult),
                     reads=nAB[2:4] + ["rsU%d" % u2], writes=["kmTa%d_%d" % (hp, c)])
                S.op("dve", lambda e, u2=u2, h0=h0, cs=cs: e.scalar_tensor_tensor(out=kmT[64:96, h0:h0 + 2, cs],
                                                                                   in0=krope[64:96, cs].unsqueeze(1).to_broadcast([32, 2, 512]),
                                                                                   scalar=cols[64:96, GMK:GMK + 1], in1=rsU[u2][64:96], op0=ALU.mult, op1=ALU.mult),
                     reads=["rsU%d" % u2], writes=["kmTb%d_%d" % (hp, c)])
                ui += 1
                mla_units.append(S.end_unit())
        S.emit_units(mla_units)
        if "qmT" in dbg:
            dbg_out["qmT"] = (qmT, [96, 8, S_])
            dbg_out["kmT"] = (kmT, [96, 8, S_])
        S.barrier()
        A.release(mMp)
        if stop_after == "MLAprep":
            return _finish(nc, es, S, A, dbg_out, outT_d)
        vm = A.alloc("vm", [128, NT, 8, 65], BF16)
        wgn = A.alloc_top("wgn", [128, 8, D], BF16)
        wgm = A.alloc_top("wgm", [128, 8, D], BF16)
        S.op("pool", lambda e: e.memset(vm[:, :, :, 64:65], 1.0), writes=["vm_ones"])
        wv3 = wukv[:].rearrange("p (h d) -> p h d", d=128)
        for t in range(NT):
            b = pb[3 + 4 * (t % 2)]
            S.op("pe", lambda e, b=b, t=t: e.matmul(b[:], lhsT=kvlatT[:, t * 128:(t + 1) * 128], rhs=wv3[:, :, 64:128], start=True, stop=True),
                 reads=["wukv"], writes=[PB[3 + 4 * (t % 2)]])
            S.op("act", lambda e, b=b, t=t: e.copy(out=vm[:, t, :, 0:64], in_=b[:].rearrange("p (h d) -> p h d", d=64)),
                 reads=[PB[3 + 4 * (t % 2)]], writes=["vm%d" % t])
        if "vm" in dbg:
            dbg_out["vm"] = (vm, [128, NT, 8, 65])
        S.op("pool", lambda e: e.dma_start(out=wgn[:], in_=kc(w_in_d[:, C_GN:C_GM])), writes=["wgn"], dma_key="wgn")
        S.op("pool", lambda e: e.dma_start(out=wgm[:], in_=kc(w_in_d[:, C_GM:IN_COLS])), writes=["wgm"], dma_key="wgm")
        pTm = [A.alloc("pTm%d" % i, [128, 512], BF16) for i in range(3)]
        rdm = [A.alloc("rdm%d" % i, [128, 4], F32) for i in range(2)]
        ytokM = A.alloc("ytokM", [128, NT, 512], BF16)
        steps = [(h, c, j) for h in range(8) for c in range(NC4) for j in range(NT)]

        def m_qk(i):
            h, c, j = steps[i]
            b3 = i % 3
            S.op("pe", lambda e: e.matmul(pb[b3][:], lhsT=kmT[:, h, j * 128:(j + 1) * 128], rhs=qmT[:, h, c * 512:(c + 1) * 512], start=True, stop=True),
                 reads=[], writes=[PB[b3]])
            S.op("act", lambda e: e.activation(out=pTm[b3][:], in_=pb[b3][:], func=AF.Exp), reads=[PB[b3]], writes=["pTm%d" % b3])

        def m_pv(i):
            h, c, j = steps[i]
            b3 = i % 3
            u = (h * NC4 + c) % 2
            ob = pb[4 + u]

            def fn(e):
                ins = None
                for qs in range(4):
                    ins = e.matmul(ob[:, qs * 65:(qs + 1) * 65], lhsT=pTm[b3][:, qs * 128:(qs + 1) * 128], rhs=vm[:, j, h, :],
                                   start=(j == 0 and qs == 0), stop=(j == NT - 1), skip_group_check=True)
                return ins
            S.op("pe", fn, reads=["pTm%d" % b3, "vm%d" % j, "vm_ones"], writes=[PB[4 + u]])
            if j == NT - 1:
                ov = ob[:, 0:260].rearrange("p (q d) -> p q d", d=65)
                S.op("dve", lambda e: e.reciprocal(out=rdm[u][:], in_=ov[:, :, 64]), reads=[PB[4 + u]], writes=["rdm%d" % u])
                S.op("dve", lambda e: e.tensor_tensor(out=ytokM[:, c * 4:(c + 1) * 4, h * 64:(h + 1) * 64], in0=ov[:, :, 0:64],
                                                      in1=rdm[u][:].unsqueeze(2).to_broadcast([128, 4, 64]), op=ALU.mult),
                     reads=[PB[4 + u], "rdm%d" % u], writes=["ytokM_%d_%d" % (h, c)])

        trb = pb[6][:].bitcast(BF16)
        trb2 = pb[7][:].bitcast(BF16)

        def emit_tr(c):
            for t in range(c * 4, c * 4 + 4):
                tb = trb if t % 2 == 0 else trb2

                def fnt(e, t=t, tb=tb):
                    ins = None
                    for bq in range(4):
                        ins = e.transpose(out=tb[:, bq * 128:(bq + 1) * 128], in_=ytokM[:, t, bq * 128:(bq + 1) * 128], identity=ident[:])
                    return ins
                S.op("pe", fnt, reads=["ytokM_%d_%d" % (h, t // 4) for h in range(8)], writes=[PB[6 + t % 2]])
                S.op("act", lambda e, t=t, tb=tb: e.copy(out=yT_mla[:, :, t * 128:(t + 1) * 128], in_=tb[:, 0:512].rearrange("p (b n) -> p b n", n=128)),
                     reads=[PB[6 + t % 2]], writes=["yT_mla%d" % t])

        ns = len(steps)
        m_qk(0)
        m_qk(1)
        for i in range(ns):
            if i + 2 < ns:
                m_qk(i + 2)
            m_pv(i)
            h_, c_, j_ = steps[i]
            if h_ == 7 and c_ >= 1 and j_ == 3:
                emit_tr(c_ - 1)
        emit_tr(NC4 - 1)
        if "yT_mla" in dbg:
            dbg_out["yT_mla"] = (yT_mla, [128, 4, S_])
        S.barrier()
        A.release(mM)
        if stop_after == "MLA":
            return _finish(nc, es, S, A, dbg_out, outT_d)

        mG = A.mark()
        mergedT = A.alloc("mergedT", [128, 8, S_], BF16)
        wout = A.alloc("wout", [128, 8, D], BF16)
        mG1 = A.mark()
        wpn = A.alloc("wpn", [128, 4, D], BF16)
        wpm = A.alloc("wpm", [128, 4, D], BF16)
        sgn = [A.alloc("sgn%d" % i, [128, 512], F32) for i in range(2)]
        sgm = [A.alloc("sgm%d" % i, [128, 512], F32) for i in range(2)]
        mm1 = [A.alloc("mm1_%d" % i, [128, 512], F32) for i in range(2)]
        mm2 = [A.alloc("mm2_%d" % i, [128, 512], F32) for i in range(2)]
        S.op("pool", lambda e: e.dma_start(out=wpn[:], in_=kc(w_pn_d)), writes=["wpn"], dma_key="wpn")
        S.op("pool", lambda e: e.dma_start(out=wpm[:], in_=kc(w_pm_d)), writes=["wpm"], dma_key="wpm")
        S.op("pool", lambda e: e.dma_start(out=wout[:], in_=kc(w_out_d)), writes=["wout"], dma_key="wout")
        ui = 0
        for c in range(NC4):
            cs = slice(c * 512, (c + 1) * 512)
            for j in range(8):
                u2 = ui % 2
                banks = [pb[u2 * 4 + i] for i in range(4)]
                names = [PB[u2 * 4 + i] for i in range(4)]
                js = slice(j * 128, (j + 1) * 128)

                def mk(bank, w, src, nk, js=js, cs=cs):
                    def fn(e):
                        ins = None
                        for k in range(nk):
                            ins = e.matmul(bank[:], lhsT=w[:, k, js], rhs=src[:, k, cs], start=(k == 0), stop=(k == nk - 1))
                        return ins
                    return fn
                S.op("pe", mk(banks[0], wgn, hT, 8), reads=["wgn"], writes=[names[0]])
                S.op("pe", mk(banks[1], wgm, hT, 8), reads=["wgm"], writes=[names[1]])
                S.op("pe", mk(banks[2], wpn, yT_na, 4), reads=["wpn"], writes=[names[2]])
                S.op("pe", mk(banks[3], wpm, yT_mla, 4), reads=["wpm"], writes=[names[3]])
                S.op("act", lambda e, u2=u2, b=banks[0]: e.activation(out=sgn[u2][:], in_=b[:], func=AF.Sigmoid), reads=[names[0]], writes=["sgn%d" % u2])
                S.op("act", lambda e, u2=u2, b=banks[1]: e.activation(out=sgm[u2][:], in_=b[:], func=AF.Sigmoid), reads=[names[1]], writes=["sgm%d" % u2])
                S.op("dve", lambda e, u2=u2, b=banks[2]: e.tensor_tensor(out=mm1[u2][:], in0=b[:], in1=sgn[u2][:], op=ALU.mult),
                     reads=[names[2], "sgn%d" % u2], writes=["mm1_%d" % u2])
                S.op("dve", lambda e, u2=u2, b=banks[3]: e.tensor_tensor(out=mm2[u2][:], in0=b[:], in1=sgm[u2][:], op=ALU.mult),
                     reads=[names[3], "sgm%d" % u2], writes=["mm2_%d" % u2])
                S.op("pool", lambda e, u2=u2, j=j, cs=cs: e.tensor_tensor(out=mergedT[:, j, cs], in0=mm1[u2][:], in1=mm2[u2][:], op=ALU.add),
                     reads=["mm1_%d" % u2, "mm2_%d" % u2], writes=["mergedT_%d_%d" % (j, c)])
                ui += 1
        if "mergedT" in dbg:
            dbg_out["mergedT"] = (mergedT, [128, 8, S_])
        S.barrier()
        A.release(mG1)
        A.release_top()
        if stop_after == "MERGE1":
            return _finish(nc, es, S, A, dbg_out, outT_d)
        for k in range(8):
            S.op("sp", lambda e, k=k: e.dma_start(out=xT[:, k, :], in_=xT_d[k * 128:(k + 1) * 128, :]), writes=["xT_%d" % k], dma_key="xTl%d" % k)
        ui = 0
        for c in range(NC4):
            cs = slice(c * 512, (c + 1) * 512)
            for dc_ in range(8):
                b = pb[ui % 4]

                def fn(e, b=b, dc_=dc_, cs=cs):
                    ins = None
                    for k in range(8):
                        ins = e.matmul(b[:], lhsT=wout[:, k, dc_ * 128:(dc_ + 1) * 128], rhs=mergedT[:, k, cs], start=(k == 0), stop=(k == 7))
                    return ins
                S.op("pe", fn, reads=["wout"], writes=[PB[ui % 4]])
                S.op("dve", lambda e, b=b, dc_=dc_, cs=cs: e.scalar_tensor_tensor(out=xT[:, dc_, cs], in0=b[:], scalar=modc[:, GT1 + dc_:GT1 + dc_ + 1],
                                                                                    in1=xT[:, dc_, cs], op0=ALU.mult, op1=ALU.add),
                     reads=[PB[ui % 4], "xT_%d" % dc_], writes=["x1_%d_%d" % (dc_, c)])
                ui += 1
        if "x1T" in dbg:
            dbg_out["x1T"] = (xT, [128, 8, S_])
        S.barrier()
        A.release(mG)
        if stop_after == "MERGE":
            return _finish(nc, es, S, A, dbg_out, outT_d)

        h2T = A.alloc("h2T", [128, 8, S_], BF16)
        gatesT = A.alloc("gatesT", [64, S_], BF16)
        gT_dram = nc.dram_tensor("gT_scratch", [E, S_], BF16, kind="Internal").ap()
        gbs = [A.alloc("gbs%d" % i, [128, S_], BF16) for i in range(4)]
        wgu = [A.alloc("wgu%d" % i, [128, 8, 512], BF16) for i in range(2)]
        wdn = [A.alloc("wdn%d" % i, [128, 2, D], BF16) for i in range(2)]
        aT = [[A.alloc("aT%d_%d" % (i, j), [128, 2, 512], BF16) for j in range(2)] for i in range(2)]
        sgs = [A.alloc("sgs%d" % i, [128, 512], BF16) for i in range(2)]
        tts = [A.alloc("tts%d" % i, [128, 512], BF16) for i in range(2)]
        GT_ALL = ["gatesT%d" % c for c in range(NC4)]

        pos_of = lambda e_: 0 if e_ == E else e_ + 1
        slot_of = lambda e_: pos_of(e_) % 4

        def load_expert(e_):
            sl = slot_of(e_)
            if e_ < E:
                srcs = (w_eg_d[e_], w_eu_d[e_], w_ed_d[e_])
            else:
                srcs = (w_sg_d, w_su_d, w_sd_d)
            rd = GT_ALL if pos_of(e_) in (2, 3) else []
            S.op("pool", lambda e: e.dma_start(out=wgu[sl][:, :, 0:256], in_=kc(srcs[0])), reads=rd, writes=["wgu%d" % sl], dma_key="wslg%d" % sl)
            S.op("pool", lambda e: e.dma_start(out=wgu[sl][:, :, 256:512], in_=kc(srcs[1])), reads=rd, writes=["wgu%d" % sl], dma_key="wslg%d" % sl)
            S.op("pool", lambda e: e.dma_start(out=wdn[sl][:], in_=kc(srcs[2])), reads=rd, writes=["wdn%d" % sl], dma_key="wsld%d" % sl)
        load_expert(E)
        load_expert(0)
        mP = A.mark()
        scores = A.alloc("scores", [128, NT, E], F32)
        wr = A.alloc("wr", [128, 8, E], F32)
        ebc = A.alloc("ebc", [128, E], F32)
        mP2 = A.mark()
        h2f = A.alloc("h2f", [128, 3, 512], F32)
        sqb2 = [A.alloc("sqb2_%d" % i, [128, 512], BF16) for i in range(2)]
        rs2 = [A.alloc("rs2_%d" % i, [128, 512], F32) for i in range(2)]
        rinv2 = [A.alloc("rinv2_%d" % i, [128, 512], F32) for i in range(2)]
        tmp3 = [A.alloc("tmp3_%d" % i, [128, 512], F32) for i in range(2)]
        S.op("sp", lambda e: e.dma_start(out=wr[:], in_=kc(w_r_d)), writes=["wr"], dma_key="wr")
        S.op("sp", lambda e: e.dma_start(out=ebc[:], in_=ebias_d.to_broadcast([128, E])), writes=["ebc"], dma_key="ebc")
        prep_units = []
        for c in range(NC4):
            S.begin_unit()
            cs = slice(c * 512, (c + 1) * 512)
            c2 = c % 2
            for k in range(8):
                S.op("act", lambda e, k=k, cs=cs: e.activation(out=sqb2[k % 2][:], in_=xT[:, k, cs], func=AF.Square), writes=["sqb2_%d" % (k % 2)])
                S.op("pe", lambda e, k=k, c2=c2: e.matmul(pb[c2][:], lhsT=ones[:], rhs=sqb2[k % 2][:], start=(k == 0), stop=(k == 7)),
                     reads=["sqb2_%d" % (k % 2)], writes=[PB[c2]])
            rinv_from(pb[c2][:], rs2[c2][:], rinv2[c2][:], D, [PB[c2]], "rinv2_%d" % c2)
            for k in range(8):
                kk = (c * 8 + k) % 3
                S.op("dve", lambda e, k=k, cs=cs, c2=c2: e.scalar_tensor_tensor(out=tmp3[k % 2][:], in0=xT[:, k, cs], scalar=A2[:, k:k + 1], in1=rinv2[c2][:],
                                                                                 op0=ALU.mult, op1=ALU.mult), reads=["rinv2_%d" % c2], writes=["tmp3_%d" % (k % 2)])
                S.op("act", lambda e, k=k, kk=kk: e.activation(out=h2f[:, kk, :], in_=tmp3[k % 2][:], func=AF.Identity, bias=modc[:, SH2 + k:SH2 + k + 1], scale=1.0),
                     reads=["tmp3_%d" % (k % 2)], writes=["h2f_%d" % kk])
                S.op("dve", lambda e, k=k, kk=kk, cs=cs: e.tensor_copy(out=h2T[:, k, cs], in_=h2f[:, kk, :]), reads=["h2f_%d" % kk], writes=["h2T_%d_%d" % (c, k)])

                def fr(e, k=k, kk=kk, c2=c2):
                    ins = None
                    for t4 in range(4):
                        ins = e.matmul(pb[2 + c2][:, t4 * E:(t4 + 1) * E], lhsT=h2f[:, kk, t4 * 128:(t4 + 1) * 128], rhs=wr[:, k, :],
                                       start=(k == 0 and t4 == 0), stop=(k == 7), skip_group_check=True)
                    return ins
                S.op("pe", fr, reads=["h2f_%d" % kk, "wr"], writes=[PB[2 + c2]])
            S.op("act", lambda e, c=c, c2=c2: e.activation(out=scores[:, c * 4:(c + 1) * 4, :], in_=pb[2 + c2][:, 0:256].rearrange("p (t x) -> p t x", x=E),
                                                    func=AF.Sigmoid), reads=[PB[2 + c2]], writes=["scores%d" % c])
            prep_units.append(S.end_unit())
        S.emit_units(prep_units)
        SC = ["scores%d" % c for c in range(NC4)]
        S.barrier()
        A.release(mP2)
        sel = A.alloc("sel", [128, NT, E], F32)
        eqm = A.alloc("bufA", [128, NT, E], F32)
        sel2 = A.alloc("bufB", [128, NT, E], F32)
        cmpb = A.alloc("cmpb", [128, NT, 8, 8], F32)
        selm = A.alloc("selm", [128, NT, E], F32)
        emask, wsel = eqm, sel2
        gates = nc.alloc_sbuf_tensor_at("gates_al", [128, NT, E], F32, offset=A.mark() - 2 * 4096)
        m1 = A.alloc("m1", [128, NT * 8], F32)
        m2 = A.alloc("m2", [128, NT * 8], F32)
        gs = A.alloc("gs", [128, NT * 8], F32)
        cnt = A.alloc("cnt", [128, NT * 8], F32)
        gmask = A.alloc("gmask", [128, NT * 8], F32)
        pen = A.alloc("pen", [128, NT * 8], F32)
        t8 = A.alloc("t8", [128, NT, 8], F32)
        wsum = A.alloc("wsum", [128, NT], F32)
        rw = A.alloc("rw", [128, NT], F32)
        v8 = lambda t_: t_[:].rearrange("p t (g x) -> p (t g) x", x=8)
        bc8 = lambda t_: t_[:].unsqueeze(2).to_broadcast([128, NT * 8, 8])
        S.op("dve", lambda e: e.tensor_tensor(out=sel[:], in0=scores[:], in1=ebc[:].unsqueeze(1).to_broadcast([128, NT, E]), op=ALU.add),
             reads=SC + ["ebc"], writes=["sel"])
        S.op("dve", lambda e: e.tensor_reduce(out=m1[:], in_=v8(sel), axis=AX.X, op=ALU.max), reads=["sel"], writes=["m1"])
        S.op("dve", lambda e: e.tensor_tensor(out=v8(eqm), in0=v8(sel), in1=bc8(m1), op=ALU.is_equal), reads=["sel", "m1"], writes=["bufA"])
        S.op("dve", lambda e: e.scalar_tensor_tensor(out=sel2[:], in0=eqm[:], scalar=-10.0, in1=sel[:], op0=ALU.mult, op1=ALU.add),
             reads=["bufA", "sel"], writes=["bufB"])
        S.op("dve", lambda e: e.tensor_reduce(out=m2[:], in_=v8(sel2), axis=AX.X, op=ALU.max), reads=["bufB"], writes=["m2"])
        S.op("dve", lambda e: e.tensor_tensor(out=gs[:], in0=m1[:], in1=m2[:], op=ALU.add), reads=["m1", "m2"], writes=["gs"])
        gs3 = gs[:].rearrange("p (t g) -> p t g", g=8)
        S.op("dve", lambda e: e.tensor_tensor(out=cmpb[:], in0=gs3.unsqueeze(2).to_broadcast([128, NT, 8, 8]),
                                              in1=gs3.unsqueeze(3).to_broadcast([128, NT, 8, 8]), op=ALU.is_gt), reads=["gs"], writes=["cmpb"])
        S.op("dve", lambda e: e.tensor_reduce(out=cnt[:], in_=cmpb[:].rearrange("p t g x -> p (t g) x"), axis=AX.X, op=ALU.add),
             reads=["cmpb"], writes=["cnt"])
        S.op("dve", lambda e: e.tensor_scalar(out=gmask[:], in0=cnt[:], scalar1=3.5, scalar2=None, op0=ALU.is_lt), reads=["cnt"], writes=["gmask"])
        S.op("dve", lambda e: e.tensor_scalar(out=pen[:], in0=gmask[:], scalar1=10.0, scalar2=-10.0, op0=ALU.mult, op1=ALU.add),
             reads=["gmask"], writes=["pen"])
        S.op("dve", lambda e: e.tensor_tensor(out=v8(selm), in0=v8(sel), in1=bc8(gmask), op=ALU.mult), reads=["sel", "gmask"], writes=["selm"])
        S.op("dve", lambda e: e.tensor_tensor(out=v8(selm), in0=v8(selm), in1=bc8(pen), op=ALU.add), reads=["selm", "pen"], writes=["selm"])
        for t in range(NT):
            S.op("dve", lambda e, t=t: e.max(out=t8[:, t, :], in_=selm[:, t, :]), reads=["selm"], writes=["t8_%d" % t])
        T8 = ["t8_%d" % t for t in range(NT)]
        S.op("dve", lambda e: e.tensor_tensor(out=emask[:], in0=selm[:], in1=t8[:, :, 7:8].to_broadcast([128, NT, E]), op=ALU.is_ge),
             reads=["selm"] + T8, writes=["bufA"])
        S.op("dve", lambda e: e.tensor_tensor(out=wsel[:], in0=scores[:], in1=emask[:], op=ALU.mult), reads=["bufA"], writes=["bufB"])
        S.op("dve", lambda e: e.tensor_reduce(out=wsum[:], in_=wsel[:], axis=AX.X, op=ALU.add), reads=["bufB"], writes=["wsum"])
        S.op("dve", lambda e: e.reciprocal(out=rw[:], in_=wsum[:]), reads=["wsum"], writes=["rw"])
        S.op("dve", lambda e: e.scalar_tensor_tensor(out=gates[:], in0=wsel[:], scalar=2.5, in1=rw[:].unsqueeze(2).to_broadcast([128, NT, E]),
                                                     op0=ALU.mult, op1=ALU.mult), reads=["bufB", "rw", "cnt"], writes=["gates", "cmpb"])
        def post_route_a():
            for c in range(NC4):
                def fng(e, c=c):
                    ins = None
                    for t4 in range(4):
                        ins = e.transpose(out=pb[4 + c % 2][0:E, t4 * 128:(t4 + 1) * 128], in_=gates[:, c * 4 + t4, :], identity=identf[:])
                    return ins
                S.op("pe", fng, reads=["gates", "identf"], writes=[PB[4 + c % 2]])
                S.op("act", lambda e, c=c: e.copy(out=gatesT[:, c * 512:(c + 1) * 512], in_=pb[4 + c % 2][0:E, :]), reads=[PB[4 + c % 2]],
                     writes=["gatesT%d" % c])
        if "gates" in dbg:
            dbg_out["gates"] = (gates, [128, NT, E])
            dbg_out["h2T"] = (h2T, [128, 8, S_])
            dbg_out["gatesT"] = (gatesT, [64, S_])
        def load_gates(e_):
            S.op("sp", lambda e: e.dma_start(out=gbs[e_ % 4][:], in_=gT_dram[e_:e_ + 1, :].to_broadcast([128, S_])), reads=["gT_dram"],
                 writes=["gbs%d" % (e_ % 4)], dma_key="gbs%d" % (e_ % 4))

        def post_route():
            post_route_a()
            S.op("sp", lambda e: e.dma_start(out=gT_dram, in_=gatesT[:]), reads=GT_ALL, writes=["gT_dram"], dma_key="gTd")
            for e_ in range(4):
                load_gates(e_)
            if len(wgu) == 4:
                load_expert(1)
                load_expert(2)
        if stop_after == "ROUTE":
            post_route()
            return _finish(nc, es, S, A, dbg_out, outT_d)

        A.release(mP2)
        wgu += [A.alloc("wgu%d" % i, [128, 8, 512], BF16) for i in range(2, 4)]
        wdn += [A.alloc("wdn%d" % i, [128, 2, D], BF16) for i in range(2, 4)]
        _ng = [d[3:] for d in dbg if d.startswith("ng=")]
        NG = int(_ng[0]) if _ng else 33
        groups = [[E]] + [[2 * g, 2 * g + 1] for g in range(32)]
        groups = groups[:NG]
        stepsE = [(g, c) for g in range(len(groups)) for c in range(NC4)]
        cnts = {"u": 0, "e": 0, "y": 0}

        def up_step(si):
            g, c = stepsE[si]
            cs = slice(c * 512, (c + 1) * 512)
            par = si % 2
            for el, e_ in enumerate(groups[g]):
                sl = slot_of(e_)
                if c == 0 and e_ + 4 <= E and (e_ + 4) // 2 < len(groups) + 0:
                    pass
                ei = cnts["e"]
                cnts["e"] += 1
                gb_bank = pb[4 + ei % 2]
                H2 = ["h2T_%d_%d" % (c, k) for k in range(8)]
                for fh in range(2):
                    ui = cnts["u"]
                    cnts["u"] += 1
                    u2 = ui % 2
                    bg, bu = pb[u2], pb[2 + u2]

                    def fn(e, bank, off, sl=sl, cs=cs):
                        ins = None
                        for k in range(8):
                            ins = e.matmul(bank[:], lhsT=wgu[sl][:, k, off:off + 128], rhs=h2T[:, k, cs], start=(k == 0), stop=(k == 7))
                        return ins
                    S.op("pe", lambda e, f=fn, bg=bg, fh=fh: f(e, bg, fh * 128), reads=["wgu%d" % sl] + H2, writes=[PB[u2]])
                    S.op("pe", lambda e, f=fn, bu=bu, fh=fh: f(e, bu, 256 + fh * 128), reads=["wgu%d" % sl] + H2, writes=[PB[2 + u2]])
                    S.op("act", lambda e, bg=bg, u2=u2: e.activation(out=sgs[u2][:], in_=bg[:], func=AF.Silu), reads=[PB[u2]], writes=["sgs%d" % u2])
                    if e_ < E:
                        S.op("dve", lambda e, bu=bu, u2=u2: e.tensor_tensor(out=tts[u2][:], in0=bu[:], in1=sgs[u2][:], op=ALU.mult),
                             reads=[PB[2 + u2], "sgs%d" % u2], writes=["tts%d" % u2])
                        S.op("dve", lambda e, u2=u2, e_=e_, par=par, el=el, fh=fh, cs=cs: e.tensor_tensor(out=aT[par][el][:, fh, :], in0=tts[u2][:],
                                                                                                          in1=gbs[e_ % 4][:, cs], op=ALU.mult),
                             reads=["tts%d" % u2, "gbs%d" % (e_ % 4)], writes=["aT%d_%d_%d" % (par, el, fh)])
                    else:
                        S.op("dve", lambda e, bu=bu, u2=u2, par=par, el=el, fh=fh: e.tensor_tensor(out=aT[par][el][:, fh, :], in0=bu[:], in1=sgs[u2][:], op=ALU.mult),
                             reads=[PB[2 + u2], "sgs%d" % u2], writes=["aT%d_%d_%d" % (par, el, fh)])

        def down_step(si):
            g, c = stepsE[si]
            cs = slice(c * 512, (c + 1) * 512)
            par = si % 2
            grp = groups[g]
            for dc_ in range(8):
                yi = cnts["y"]
                cnts["y"] += 1
                yb = pb[4 + yi % 4]

                def fn(e, yb=yb, dc_=dc_):
                    ins = None
                    n = len(grp) * 2
                    i = 0
                    for el, e_ in enumerate(grp):
                        for fh in range(2):
                            ins = e.matmul(yb[:], lhsT=wdn[slot_of(e_)][:, fh, dc_ * 128:(dc_ + 1) * 128], rhs=aT[par][el][:, fh, :], start=(i == 0), stop=(i == n - 1))
                            i += 1
                    return ins
                S.op("pe", fn, reads=["wdn%d" % slot_of(e_) for e_ in grp] + ["aT%d_%d_%d" % (par, el, fh) for el in range(len(grp)) for fh in range(2)],
                     writes=[PB[4 + yi % 4]])
                S.op("dve", lambda e, yb=yb, dc_=dc_, cs=cs: e.scalar_tensor_tensor(out=xT[:, dc_, cs], in0=yb[:], scalar=modc[:, GT2 + dc_:GT2 + dc_ + 1],
                                                                                     in1=xT[:, dc_, cs], op0=ALU.mult, op1=ALU.add),
                     reads=[PB[4 + yi % 4]], writes=["x2_%d_%d" % (dc_, c)])
            if c == NC4 - 1:
                for e_ in grp:
                    p_n = pos_of(e_) + 4
                    nxt = p_n - 1
                    if p_n <= E and (nxt // 2) + 1 < len(groups):
                        load_expert(nxt)
                        if nxt >= 4:
                            load_gates(nxt)

        nsE = len(stepsE)
        for si in range(nsE):
            up_step(si)
            if si == min(1, nsE - 1):
                post_route()
            if si >= 1:
                down_step(si - 1)
        down_step(nsE - 1)
        for c in range(NC4):
            cs = slice(c * 512, (c + 1) * 512)
            for k in range(8):
                S.op("sp", lambda e, k=k, cs=cs: e.dma_start(out=outT_d[k * 128:(k + 1) * 128, cs], in_=xT[:, k, cs]),
                     reads=["x2_%d_%d" % (k, c)], dma_key="out%d" % k)
        return _finish(nc, es, S, A, dbg_out, outT_d)


def _router_mm(e, bank, h2f, wr):
    ins = None
    for t4 in range(4):
        for k in range(8):
            ins = e.matmul(bank[:, t4 * E:(t4 + 1) * E], lhsT=h2f[:, k, t4 * 128:(t4 + 1) * 128], rhs=wr[:, k, :], start=(k == 0), stop=(k == 7))
    return ins


def _finish(nc, es, S, A, dbg_out, outT_d):
    print("[build] sbuf peak bytes/partition:", A.peak, "of", A.TOP)
    S.barrier()
    for name, (t, shape) in dbg_out.items():
        dt = t.dtype
        d = nc.dram_tensor("dbg_" + name, list(shape), dt, kind="ExternalOutput").ap()
        S.op("sp", lambda e, d=d, t=t: e.dma_start(out=d, in_=t[:]), dma_key="out_dbg")
    S.finalize(es)
    with nc.Block() as block:
        S.emit(block)
    return nc


def _col(v, n):
    return np.ascontiguousarray(np.asarray(v, np.float32).reshape(n, 128).T)


def _na_table(rpb):
    rpb = np.asarray(rpb, np.float32)
    p = np.arange(128)
    kr, kcol = p // 64, p % 64
    qc = np.arange(64)
    cstart = np.clip(qc - 8, 0, 48)
    colok = (kcol[:, None] >= cstart[None, :]) & (kcol[:, None] < cstart[None, :] + 16)
    dc = np.clip(kcol[:, None] - qc[None, :], -15, 15) + 15
    tab = np.full((128, 8, 24, 64), NEG, np.float32)
    for n in range(24):
        u = 4 + n if n < 10 else 2 + (n - 10)
        dr = 8 + kr - u
        rowok = ((dr >= -4) & (dr <= 3)) if n < 10 else (np.abs(dr) <= 7)
        ok = rowok[:, None] & colok
        dri = np.clip(dr + 7, 0, 14)
        vals = rpb[:, dri[:, None], dc]
        vals = np.transpose(vals, (1, 0, 2))
        tab[:, :, n, :] = np.where(ok[:, None, :], vals, np.float32(NEG))
    return tab


def _prep_inputs(inputs):
    f = lambda k: np.asarray(inputs[k], np.float32)
    x = f("x")
    B = x.shape[0]
    half = 16
    inv_freq = (10000.0 ** (-np.arange(half, dtype=np.float32) / half)).astype(np.float32)
    ifr = np.zeros((96, 1), np.float32)
    ifr[64:80, 0] = inv_freq / np.float32(2 * np.pi)
    ifr[80:96, 0] = inv_freq / np.float32(2 * np.pi)
    pad96 = lambda v: np.ascontiguousarray(np.asarray(v, np.float32).reshape(96, 1))
    shared = {
        "w_ada": np.ascontiguousarray(f("w_ada")[0]),
        "b_col": _col(f("b_ada")[0], 48),
        "g1_col": _col(f("g_norm1")[0], 8),
        "g2_col": _col(f("g_norm2")[0], 8),
        "w_in": np.ascontiguousarray(f("w_in")[0]),
        "gnaq_col": np.ascontiguousarray(np.tile(f("g_na_q")[0], 2).reshape(128, 1)),
        "gnak_col": np.ascontiguousarray(np.tile(f("g_na_k")[0], 2).reshape(128, 1)),
        "tab": _na_table(f("na_rpb")[0]),
        "gql_col": _col(f("g_q_lat")[0], 2),
        "gkvl_col": _col(f("g_kv_lat")[0], 1),
        "w_uq": np.ascontiguousarray(f("w_uq")[0]),
        "w_ukv": np.ascontiguousarray(f("w_ukv")[0]),
        "gmq_col": pad96(f("g_mla_q")[0]),
        "gmk_col": pad96(f("g_mla_k")[0]),
        "ifr_col": ifr,
        "w_proj_na": np.ascontiguousarray(f("w_proj_na")[0]),
        "w_proj_mla": np.ascontiguousarray(f("w_proj_mla")[0]),
        "w_out": np.ascontiguousarray(f("w_out")[0]),
        "w_router": np.ascontiguousarray(f("w_router")[0]),
        "e_bias": np.ascontiguousarray(f("e_bias")[0].reshape(1, E)),
        "w_exp_gate": np.ascontiguousarray(f("w_exp_gate")[0]),
        "w_exp_up": np.ascontiguousarray(f("w_exp_up")[0]),
        "w_exp_down": np.ascontiguousarray(f("w_exp_down")[0]),
        "w_sh_gate": np.ascontiguousarray(f("w_sh_gate")[0]),
        "w_sh_up": np.ascontiguousarray(f("w_sh_up")[0]),
        "w_sh_down": np.ascontiguousarray(f("w_sh_down")[0]),
    }
    c = f("c")
    pos = np.asarray(inputs["positions"], np.int32)
    maps = []
    for b in range(B):
        m = dict(shared)
        m["xT"] = np.ascontiguousarray(x[b].T)
        m["cT"] = _col(c[b], 8)
        m["pos"] = np.ascontiguousarray(pos[b].reshape(1, S_))
        maps.append(m)
    return maps


_NC_CACHE = {}


def kernel(**inputs):
    maps = _prep_inputs(inputs)
    if "nc" not in _NC_CACHE:
        _NC_CACHE["nc"] = build_nc()
    nc = _NC_CACHE["nc"]
    res = run_bass_kernel_spmd(nc, maps, core_ids=list(range(len(maps))))
    out = np.stack([np.ascontiguousarray(r["outT"].T) for r in res.results], axis=0)
    return out.astype(np.float32)
```
